# Optimizing a Trainium2 kernel written in Bass

```python
import math
import jax, jax.numpy as jnp
from jax import lax
import numpy as np

D_MODEL = 1024
BATCH = 8
SEQ = 4096
DEPTH = 1

SB_HEADS = 8
SB_HEAD_DIM = 64
SB_WIDTH = SB_HEADS * SB_HEAD_DIM
MLA_HEADS = 8
MLA_NOPE_DIM = 64
MLA_ROPE_DIM = 32
MLA_V_DIM = 64
MLA_Q_RANK = 384
MLA_KV_RANK = 256
MLA_QK_DIM = MLA_NOPE_DIM + MLA_ROPE_DIM
MLA_WIDTH = MLA_HEADS * MLA_V_DIM
ROPE_THETA = 10000.0
Q_BLOCK = 128
N_GROUPS = 4
EXPERTS_PER_GROUP = 8
N_EXPERTS = N_GROUPS * EXPERTS_PER_GROUP
TOP_K_IN_GROUP = 2
D_EXPERT = 256
DEEPNORM_ALPHA = (2.0 * DEPTH) ** 0.25
DEEPNORM_BETA = (8.0 * DEPTH) ** -0.25
LN_EPS = 1e-5
RMS_EPS = 1e-6
IN_SPLITS = (SB_WIDTH, SB_WIDTH, SB_WIDTH, MLA_Q_RANK, MLA_KV_RANK, MLA_ROPE_DIM, D_MODEL, D_MODEL)
IN_COLS = sum(IN_SPLITS)
N_MOD = 6

kernel_name = "hybrid_sb_mla_hmoe_deepnorm_adaln"


def layer_norm(x, g, b):
    xf = x.astype(jnp.float32)
    mu = jnp.mean(xf, axis=-1, keepdims=True)
    var = jnp.mean(jnp.square(xf - mu), axis=-1, keepdims=True)
    y = (xf - mu) * lax.rsqrt(var + LN_EPS)
    return (y * g.astype(jnp.float32) + b.astype(jnp.float32)).astype(x.dtype)


def rms_norm(x, g):
    xf = x.astype(jnp.float32)
    y = xf * lax.rsqrt(jnp.mean(jnp.square(xf), axis=-1, keepdims=True) + RMS_EPS)
    return (y * g.astype(jnp.float32)).astype(x.dtype)


def rope_tables(positions):
    inv_freq = 1.0 / (ROPE_THETA ** (jnp.arange(0, MLA_ROPE_DIM, 2, dtype=jnp.float32) / MLA_ROPE_DIM))
    ang = positions.astype(jnp.float32)[..., None] * inv_freq
    return jnp.cos(ang), jnp.sin(ang)


def apply_rope(x, cos, sin):
    half = x.shape[-1] // 2
    xf = x.astype(jnp.float32)
    x1, x2 = xf[..., :half], xf[..., half:]
    cs, sn = cos[:, :, None, :], sin[:, :, None, :]
    return jnp.concatenate([x1 * cs - x2 * sn, x1 * sn + x2 * cs], axis=-1).astype(x.dtype)


def stick_breaking_attention(q, k, v):
    B, S, H, d = q.shape
    nb = S // Q_BLOCK
    scale = 1.0 / math.sqrt(d)
    q_blocks = q.reshape(B, nb, Q_BLOCK, H, d).transpose(1, 0, 2, 3, 4)
    key_idx = jnp.arange(S)

    def block(args):
        q_blk, start = args
        z = jnp.einsum('bqhd,bkhd->bhqk', q_blk, k, preferred_element_type=jnp.float32) * scale
        q_idx = start + jnp.arange(Q_BLOCK)
        mask = key_idx[None, :] < q_idx[:, None]
        log_1m_beta = jnp.where(mask, jax.nn.log_sigmoid(-z), 0.0)
        suffix = lax.cumsum(log_1m_beta, axis=3, reverse=True) - log_1m_beta
        w = jnp.where(mask, jnp.exp(jax.nn.log_sigmoid(z) + suffix), 0.0)
        return jnp.einsum('bhqk,bkhd->bqhd', w.astype(v.dtype), v)

    out = lax.map(block, (q_blocks, jnp.arange(nb) * Q_BLOCK))
    return out.transpose(1, 0, 2, 3, 4).reshape(B, S, H, d)


def mla_attention(q_nope, q_rope, k_nope, k_rope, v):
    B, S, H, dn = q_nope.shape
    dr = q_rope.shape[-1]
    nb = S // Q_BLOCK
    scale = 1.0 / math.sqrt(MLA_QK_DIM)
    qn_blocks = q_nope.reshape(B, nb, Q_BLOCK, H, dn).transpose(1, 0, 2, 3, 4)
    qr_blocks = q_rope.reshape(B, nb, Q_BLOCK, H, dr).transpose(1, 0, 2, 3, 4)
    key_idx = jnp.arange(S)

    def block(args):
        qn, qr, start = args
        s = (jnp.einsum('bqhd,bkhd->bhqk', qn, k_nope, preferred_element_type=jnp.float32)
             + jnp.einsum('bqhr,bkr->bhqk', qr, k_rope, preferred_element_type=jnp.float32)) * scale
        q_idx = start + jnp.arange(Q_BLOCK)
        mask = key_idx[None, :] <= q_idx[:, None]
        p = jax.nn.softmax(jnp.where(mask, s, -jnp.inf), axis=-1)
        return jnp.einsum('bhqk,bkhd->bqhd', p.astype(v.dtype), v)

    out = lax.map(block, (qn_blocks, qr_blocks, jnp.arange(nb) * Q_BLOCK))
    return out.transpose(1, 0, 2, 3, 4).reshape(B, S, H, v.shape[-1])


def hierarchical_moe(t, w_rg, b_rg, w_re, b_re, w_g, w_u, w_d):
    N = t.shape[0]
    group_logits = (t @ w_rg + b_rg).astype(jnp.float32)
    group_probs = jax.nn.softmax(group_logits, axis=-1)
    p_group, g_idx = lax.top_k(group_probs, 1)
    expert_logits = (t @ w_re + b_re).astype(jnp.float32).reshape(N, N_GROUPS, EXPERTS_PER_GROUP)
    sel_logits = jnp.take_along_axis(expert_logits, g_idx[:, :, None], axis=1)[:, 0]
    top_vals, top_idx = lax.top_k(sel_logits, TOP_K_IN_GROUP)
    weights = jax.nn.softmax(top_vals, axis=-1) * p_group
    expert_id = g_idx * EXPERTS_PER_GROUP + top_idx
    combine = jnp.sum(jax.nn.one_hot(expert_id, N_EXPERTS, dtype=jnp.float32) * weights[..., None],
                      axis=1).astype(t.dtype)
    out = jnp.zeros_like(t)
    for gi in range(N_GROUPS):
        sl = slice(gi * EXPERTS_PER_GROUP, (gi + 1) * EXPERTS_PER_GROUP)
        h = jax.nn.silu(jnp.einsum('nd,edf->nef', t, w_g[sl])) * jnp.einsum('nd,edf->nef', t, w_u[sl])
        h = h * combine[:, sl, None]
        out = out + jnp.einsum('nef,efd->nd', h, w_d[sl])
    return out


def setup_inputs(seed: int = 0) -> dict:
    key = jax.random.key(seed)
    ks = jax.random.split(key, 26)
    f32 = jnp.float32
    D = D_MODEL

    def nrm(k, shape, scale):
        return jax.random.normal(k, shape, f32) * scale

    x = jax.random.normal(ks[0], (BATCH, SEQ, D), f32)
    c = jax.random.normal(ks[1], (BATCH, D), f32)
    offset = jax.random.randint(ks[2], (BATCH,), 0, 1024, dtype=jnp.int32)
    positions = offset[:, None] + jnp.arange(SEQ, dtype=jnp.int32)[None, :]

    col_scale = np.ones((IN_COLS,), np.float32)
    col_scale[2 * SB_WIDTH:3 * SB_WIDTH] = DEEPNORM_BETA
    w_in = nrm(ks[5], (DEPTH, D, IN_COLS), D ** -0.5) * jnp.asarray(col_scale)

    return {
        "x": x,
        "c": c,
        "positions": positions,
        "w_ada": nrm(ks[3], (DEPTH, D, N_MOD * D), D ** -0.5),
        "b_ada": nrm(ks[4], (DEPTH, N_MOD * D), 0.02),
        "w_in": w_in,
        "mla_q_norm_g": 1.0 + nrm(ks[6], (DEPTH, MLA_Q_RANK), 0.02),
        "w_q_up": nrm(ks[7], (DEPTH, MLA_Q_RANK, MLA_HEADS * MLA_QK_DIM), MLA_Q_RANK ** -0.5),
        "mla_kv_norm_g": 1.0 + nrm(ks[8], (DEPTH, MLA_KV_RANK), 0.02),
        "w_kv_up": nrm(ks[9], (DEPTH, MLA_KV_RANK, MLA_HEADS * (MLA_NOPE_DIM + MLA_V_DIM)), MLA_KV_RANK ** -0.5),
        "w_branch_sb": nrm(ks[10], (DEPTH, SB_WIDTH, D), SB_WIDTH ** -0.5 * DEEPNORM_BETA),
        "w_branch_mla": nrm(ks[11], (DEPTH, MLA_WIDTH, D), MLA_WIDTH ** -0.5 * DEEPNORM_BETA),
        "w_out": nrm(ks[12], (DEPTH, D, D), D ** -0.5 * DEEPNORM_BETA),
        "ln1_g": 1.0 + nrm(ks[13], (DEPTH, D), 0.02),
        "ln1_b": nrm(ks[14], (DEPTH, D), 0.02),
        "w_router_group": nrm(ks[15], (DEPTH, D, N_GROUPS), D ** -0.5),
        "b_router_group": nrm(ks[16], (DEPTH, N_GROUPS), 0.01),
        "w_router_expert": nrm(ks[17], (DEPTH, D, N_EXPERTS), D ** -0.5),
        "b_router_expert": nrm(ks[18], (DEPTH, N_EXPERTS), 0.01),
        "w_exp_gate": nrm(ks[19], (DEPTH, N_EXPERTS, D, D_EXPERT), D ** -0.5),
        "w_exp_up": nrm(ks[20], (DEPTH, N_EXPERTS, D, D_EXPERT), D ** -0.5 * DEEPNORM_BETA),
        "w_exp_down": nrm(ks[21], (DEPTH, N_EXPERTS, D_EXPERT, D), D_EXPERT ** -0.5 * DEEPNORM_BETA),
        "ln2_g": 1.0 + nrm(ks[22], (DEPTH, D), 0.02),
        "ln2_b": nrm(ks[23], (DEPTH, D), 0.02),
    }


def reference(x, c, positions, w_ada, b_ada, w_in, mla_q_norm_g, w_q_up, mla_kv_norm_g, w_kv_up,
              w_branch_sb, w_branch_mla, w_out, ln1_g, ln1_b, w_router_group, b_router_group,
              w_router_expert, b_router_expert, w_exp_gate, w_exp_up, w_exp_down, ln2_g, ln2_b):
    B, S, D = x.shape
    cos, sin = rope_tables(positions)
    c_act = jax.nn.silu(c)
    split_points = []
    acc = 0
    for width in IN_SPLITS[:-1]:
        acc += width
        split_points.append(acc)

    for l in range(DEPTH):
        mod = c_act @ w_ada[l] + b_ada[l]
        shift1, scale1, gate1, shift2, scale2, gate2 = [m[:, None, :] for m in jnp.split(mod, N_MOD, axis=-1)]

        u = x * (1.0 + scale1) + shift1
        proj = u @ w_in[l]
        q_sb, k_sb, v_sb, q_down, kv_down, k_rope_raw, g_sb, g_mla = jnp.split(proj, split_points, axis=-1)

        o_sb = stick_breaking_attention(q_sb.reshape(B, S, SB_HEADS, SB_HEAD_DIM),
                                        k_sb.reshape(B, S, SB_HEADS, SB_HEAD_DIM),
                                        v_sb.reshape(B, S, SB_HEADS, SB_HEAD_DIM))

        q = (rms_norm(q_down, mla_q_norm_g[l]) @ w_q_up[l]).reshape(B, S, MLA_HEADS, MLA_QK_DIM)
        q_nope = q[..., :MLA_NOPE_DIM]
        q_rope = apply_rope(q[..., MLA_NOPE_DIM:], cos, sin)
        kv = (rms_norm(kv_down, mla_kv_norm_g[l]) @ w_kv_up[l]).reshape(B, S, MLA_HEADS, MLA_NOPE_DIM + MLA_V_DIM)
        k_nope = kv[..., :MLA_NOPE_DIM]
        v_mla = kv[..., MLA_NOPE_DIM:]
        k_rope = apply_rope(k_rope_raw[:, :, None, :], cos, sin)[:, :, 0, :]
        o_mla = mla_attention(q_nope, q_rope, k_nope, k_rope, v_mla)

        y_sb = o_sb.reshape(B, S, SB_WIDTH) @ w_branch_sb[l]
        y_mla = o_mla.reshape(B, S, MLA_WIDTH) @ w_branch_mla[l]
        mixed = jax.nn.sigmoid(g_sb) * y_sb + jax.nn.sigmoid(g_mla) * y_mla
        attn_out = mixed @ w_out[l]
        x = layer_norm(DEEPNORM_ALPHA * x + gate1 * attn_out, ln1_g[l], ln1_b[l])

        u2 = x * (1.0 + scale2) + shift2
        moe = hierarchical_moe(u2.reshape(B * S, D), w_router_group[l], b_router_group[l],
                               w_router_expert[l], b_router_expert[l],
                               w_exp_gate[l], w_exp_up[l], w_exp_down[l]).reshape(B, S, D)
        x = layer_norm(DEEPNORM_ALPHA * x + gate2 * moe, ln2_g[l], ln2_b[l])
    return x
```

```python
import contextlib
import numpy as np
import concourse.bass as bass
import concourse.mybir as mybir
from concourse.bass_utils import run_bass_kernel_spmd

F32 = mybir.dt.float32
BF16 = mybir.dt.bfloat16
I32 = mybir.dt.int32
ALU = mybir.AluOpType
AF = mybir.ActivationFunctionType
AX = mybir.AxisListType

S = 4096
D = 1024
NT = 32
NG = 8
C_QSB, C_KSB, C_VSB, C_QD, C_KVD, C_KR, C_GSB, C_GMLA = 0, 512, 1024, 1536, 1920, 2176, 2208, 3232
ALPHA = 2.0 ** 0.25
MAGIC = 12582912.0
TWO_PI = 6.283185307179586
NEG_BIG = -30000.0
SAME_ENGINE_SYNC = True
LIMIT = None
PAD = 0
START = 0
MLIMIT = None
DBGM = False
NOFIN = False
SK2, SK3 = 1, 2


class Buf:
    __slots__ = ("t", "w", "r", "ds", "sw", "name")

    def __init__(self, t, name=""):
        self.t = t
        self.w = None
        self.r = {}
        self.ds = None
        self.sw = None
        self.name = name

    def __getitem__(self, k):
        return self.t[k]


class DSem:
    __slots__ = ("sem", "count")

    def __init__(self, sem):
        self.sem = sem
        self.count = 0


class KB:
    def __init__(self):
        nc = bass.Bass("TRN2", target_bir_lowering=False)
        self.nc = nc
        self.eng = {"pe": nc.tensor, "act": nc.scalar, "dve": nc.vector, "pool": nc.gpsimd, "sp": nc.sync}
        self.root = contextlib.ExitStack()
        self.esem = {}
        self.tick = {}
        for e in ("pe", "act", "dve", "pool"):
            self.esem[e] = self.root.enter_context(nc.semaphore("es_" + e))
            self.tick[e] = 0
        self.waited = {e: {} for e in self.eng}
        self.dpool = [DSem(self.root.enter_context(nc.semaphore("ds%d" % i))) for i in range(80)]
        self.swpool = [DSem(self.root.enter_context(nc.semaphore("sw%d" % i))) for i in range(8)]
        self.swnext = 0
        self.dnext = 0
        self.dbase = None
        self.uid = 0

    def sb(self, st, shape, dt, name=None):
        self.uid += 1
        name = "%s_%d" % (name or "t", self.uid)
        return Buf(st.enter_context(self.nc.sbuf_tensor(name, shape, dt)), name)

    def ps(self, st, shape, dt, name=None):
        self.uid += 1
        name = "%s_%d" % (name or "p", self.uid)
        return Buf(st.enter_context(self.nc.psum_tensor(name, shape, dt)), name)

    def view(self, buf, name=""):
        return Buf(buf.t, name)

    def new_phase(self):
        if self.dbase is None:
            self.dbase = self.dnext
        self.dnext = self.dbase

    def _dsem(self, b):
        if b.ds is None:
            b.ds = self.dpool[self.dnext]
            self.dnext += 1
            if self.dnext >= len(self.dpool):
                self.dnext = self.dbase or 0
        return b.ds

    def _sync(self, e, reads, writes):
        need = {}
        wd = self.waited[e]

        def add(m):
            if m is None:
                return
            sem, val, src = m
            if src == e and (e == "pe" or not SAME_ENGINE_SYNC):
                return
            key = id(sem)
            if wd.get(key, 0) >= val:
                return
            if key not in need or need[key][1] < val:
                need[key] = (sem, val)

        for b in reads:
            add(b.w)
        for b in writes:
            add(b.w)
            for m in b.r.values():
                add(m)
        for key, (sem, val) in need.items():
            self.eng[e].wait_ge(sem, val)
            wd[key] = val

    def op(self, e, fn, reads=(), writes=(), signal=True):
        self._sync(e, reads, writes)
        ins = fn(self.eng[e])
        if signal:
            self.tick[e] += 1
            ins.then_inc(self.esem[e], 1)
            m = (self.esem[e], self.tick[e], e)
        else:
            m = (self.esem[e], self.tick[e] + 1, e)
        k = id(m[0])
        for b in reads:
            b.r[k] = m
        for b in writes:
            b.w = m
            b.r = {}
        return ins

    def dma(self, q, out, in_, reads=(), writes=()):
        self._sync(q, reads, writes)
        ins = self.eng[q].dma_start(out=out, in_=in_)
        owner = writes[0] if writes else reads[0]
        if q == "pool":
            if owner.sw is None:
                owner.sw = self.swpool[self.swnext]
                self.swnext += 1
            ds = owner.sw
        else:
            ds = self._dsem(owner)
        ds.count += 16
        ins.then_inc(ds.sem, 16)
        m = (ds.sem, ds.count, "dma")
        k = id(ds.sem)
        for b in reads:
            b.r[k] = m
        for b in writes:
            b.w = m
            b.r = {}
        return ins

    def barrier(self):
        for e in self.eng:
            for o in ("pe", "act", "dve", "pool"):
                if o != e and self.tick[o] > self.waited[e].get(id(self.esem[o]), 0):
                    self.eng[e].wait_ge(self.esem[o], self.tick[o])
                    self.waited[e][id(self.esem[o])] = self.tick[o]
            for ds in self.dpool + self.swpool:
                if ds.count > self.waited[e].get(id(ds.sem), 0):
                    self.eng[e].wait_ge(ds.sem, ds.count)
                    self.waited[e][id(ds.sem)] = ds.count

    def mm(self, out_ap, pairs, outb, inbs, start=True, stop=True, signal=True):
        n = len(pairs)
        for i, (l, r) in enumerate(pairs):
            st = start and i == 0
            sp = stop and i == n - 1
            self.op("pe", lambda pe, l=l, r=r, st=st, sp=sp: pe.matmul(out_ap, lhsT=l, rhs=r, start=st, stop=sp),
                    reads=inbs if i == 0 else (), writes=[outb] if i == 0 else (),
                    signal=(signal and i == n - 1))
        if n and not signal:
            pass

    def tr(self, out_ap, in_ap, ident_ap, outb, inbs, signal=True):
        self.op("pe", lambda pe: pe.transpose(out_ap, in_ap, ident_ap), reads=inbs, writes=[outb], signal=signal)

    def act(self, out, in_, func, outb, inbs, scale=1.0, bias=None, accum_out=None, extra_w=()):
        kw = {}
        if bias is not None:
            kw["bias"] = bias
        if accum_out is not None:
            kw["accum_out"] = accum_out
        return self.op("act", lambda a: a.activation(out=out, in_=in_, func=func, scale=scale, **kw),
                       reads=inbs, writes=[outb] + list(extra_w))

    def tt(self, e, out, in0, in1, op, outb, inbs):
        return self.op(e, lambda v: v.tensor_tensor(out=out, in0=in0, in1=in1, op=op), reads=inbs, writes=[outb])

    def ts(self, out, in0, s1, s2, op0, op1, outb, inbs):
        if op1 is None:
            return self.op("dve", lambda v: v.tensor_single_scalar(out=out, in_=in0, scalar=s1, op=op0),
                           reads=inbs, writes=[outb])
        return self.op("dve", lambda v: v.tensor_scalar(out=out, in0=in0, scalar1=s1, scalar2=s2, op0=op0, op1=op1),
                       reads=inbs, writes=[outb])

    def cp(self, e, out, in_, outb, inbs):
        if e == "act":
            return self.act(out, in_, AF.Copy, outb, inbs)
        return self.op(e, lambda v: v.tensor_copy(out, in_), reads=inbs, writes=[outb])

    def memset(self, e, ap, val, outb):
        return self.op(e, lambda v: v.memset(ap, val), reads=(), writes=[outb])


def _rr(lst):
    i = [0]

    def nxt():
        b = lst[i[0] % len(lst)]
        i[0] += 1
        return b
    return nxt


def build(debug=None):
    k = KB()
    nc = k.nc
    es = k.root
    dbg = debug is not None
    skind = "ExternalOutput" if dbg else "Internal"

    def din(name, shape, dt=F32):
        return nc.dram_tensor(name, shape, dt, kind="ExternalInput").ap()

    def dsc(name, shape, dt):
        return nc.dram_tensor(name, shape, dt, kind=skind).ap()

    x_d = din("x", [S, D])
    c_d = din("c", [1, D])
    pos_d = din("positions", [1, S], I32)
    cst_d = din("cst", [128, 4])
    w_ada_d = din("w_ada", [D, 6 * D])
    b_ada_d = din("b_ada", [1, 6 * D])
    w_in_d = din("w_in", [D, 4256])
    gq_d = din("mla_q_norm_g", [1, 384])
    w_qup_d = din("w_q_up", [384, 768])
    gkv_d = din("mla_kv_norm_g", [1, 256])
    w_kvup_d = din("w_kv_up", [256, 1024])
    w_bsb_d = din("w_branch_sb", [512, D])
    w_bmla_d = din("w_branch_mla", [512, D])
    w_out_d = din("w_out", [D, D])
    ln1g_d = din("ln1_g", [1, D])
    ln1b_d = din("ln1_b", [1, D])
    w_rg_d = din("w_router_group", [D, 4])
    b_rg_d = din("b_router_group", [1, 4])
    w_re_d = din("w_router_expert", [D, 32])
    b_re_d = din("b_router_expert", [1, 32])
    NE_DECL = 1 if (dbg and debug != "C") else 32
    w_eg_d = din("w_exp_gate", [NE_DECL, D, 256])
    w_eu_d = din("w_exp_up", [NE_DECL, D, 256])
    w_ed_d = din("w_exp_down", [NE_DECL, 256, D])
    ln2g_d = din("ln2_g", [1, D])
    ln2b_d = din("ln2_b", [1, D])
    out_d = nc.dram_tensor("out", [S, D], F32, kind="ExternalOutput").ap()

    mod_d = dsc("mod_s", [1, 6 * D], F32)
    cos_d = dsc("cos_s", [96, S], F32)
    sin_d = dsc("sin_s", [96, S], F32)
    qsb_d = dsc("qsb_s", [4, 128, S], BF16)
    ksb_d = dsc("ksb_s", [4, 128, S], BF16)
    vsb_d = dsc("vsb_s", [S, 512], BF16)
    qm_d = dsc("qm_s", [8, 96, S], BF16)
    km_d = dsc("km_s", [8, 96, S], BF16)
    vm_d = dsc("vm_s", [S, 640], BF16)
    o_d = dsc("o_s", [S, 1024], BF16)
    acc_d = dsc("acc_s", [S, D], F32)
    u2T_d = dsc("u2T_s", [8, 128, S], BF16)
    comb_d = dsc("comb_s", [S, 32], F32)

    st = contextlib.ExitStack()
    st.enter_context(nc.allow_non_contiguous_dma(reason="small strided setup loads"))

    identF = k.sb(es, [128, 128], F32, "identF")
    identB = k.sb(es, [128, 128], BF16, "identB")
    onesB = k.sb(es, [128, 128], BF16, "onesB")
    modT = k.sb(es, [128, 48], F32, "modT")
    sc1p = k.sb(es, [128, 8], F32, "sc1p")
    cst = k.sb(es, [128, 4], F32, "cst")

    k.memset("pool", identF[:, :], 1.0, identF)
    k.op("pool", lambda g: g.affine_select(out=identF[:, :], in_=identF[:, :], pattern=[[-1, 128]],
                                           compare_op=ALU.is_equal, fill=0.0, base=0, channel_multiplier=1),
         reads=(), writes=[identF])
    k.cp("dve", identB[:, :], identF[:, :], identB, [identF])
    k.memset("dve", onesB[:, :], 1.0, onesB)
    k.dma("sp", cst[:, :], cst_d[:, :], writes=[cst])

    with contextlib.ExitStack() as ph:
        k.new_phase()
        cT = k.sb(ph, [128, 8], F32, "cT")
        cA = k.sb(ph, [128, 8], F32, "cA")
        bada = k.sb(ph, [1, 6 * D], F32, "bada")
        modrow = k.sb(ph, [1, 6 * D], F32, "modrow")
        wa = [k.sb(ph, [128, 8, 512], F32, "wa") for _ in range(2)]
        pm = [k.ps(ph, [128, 512], F32, "pm") for _ in range(2)]
        mod_db = Buf(None, "mod_d")
        k.dma("sp", cT[:, :], c_d.rearrange("o (c p) -> p (o c)", p=128), writes=[cT])
        k.dma("sp", bada[:, :], b_ada_d[:, :], writes=[bada])
        k.act(cA[:, :], cT[:, :], AF.Silu, cA, [cT])
        wv = w_ada_d.rearrange("(c p) n -> p c n", p=128)
        for nb in range(12):
            w = wa[nb % 2]
            k.dma("sp", w[:, :, :], wv[:, :, nb * 512:(nb + 1) * 512], writes=[w])
            p = pm[nb % 2]
            k.mm(p[0:1, :], [(cA[:, c:c + 1], w[:, c, :]) for c in range(8)], p, [cA, w])
            k.tt("dve", modrow[0:1, nb * 512:(nb + 1) * 512], p[0:1, :], bada[0:1, nb * 512:(nb + 1) * 512],
                 ALU.add, modrow, [p, bada])
        k.dma("sp", mod_d[:, :], modrow[:, :], reads=[modrow], writes=[mod_db])
        k.dma("sp", modT[:, :], mod_d.rearrange("o (j p) -> p (o j)", p=128), reads=[mod_db], writes=[modT])
        k.ts(sc1p[:, :], modT[:, 8:16], 1.0, None, ALU.add, None, sc1p, [modT])

        posi = k.sb(ph, [96, S], I32, "posi")
        ang = k.sb(ph, [96, S], F32, "ang")
        t0 = k.sb(ph, [96, S], F32, "t0")
        t1 = k.sb(ph, [96, S], F32, "t1")
        k.dma("sp", posi[:, :], pos_d[0:1, :].partition_broadcast(96), writes=[posi])
        k.cp("pool", ang[:, :], posi[:, :], ang, [posi])
        k.ts(ang[:, :], ang[:, :], cst[0:96, 0:1], None, ALU.mult, None, ang, [ang, cst])
        for which in range(2):
            src = ang
            if which == 1:
                k.ts(t1[:, :], ang[:, :], TWO_PI / 4.0, None, ALU.add, None, t1, [ang])
                src = t1
            k.ts(t0[:, :], src[:, :], 1.0 / TWO_PI, MAGIC, ALU.mult, ALU.add, t0, [src])
            k.ts(t0[:, :], t0[:, :], -MAGIC, -TWO_PI, ALU.add, ALU.mult, t0, [t0])
            k.tt("dve", t0[:, :], t0[:, :], src[:, :], ALU.add, t0, [t0, src])
            k.ts(t0[:, :], t0[:, :], -3.1415925, 3.1415925, ALU.max, ALU.min, t0, [t0])
            k.act(t0[:, :], t0[:, :], AF.Sin, t0, [t0])
            if which == 0:
                k.ts(t0[:, :], t0[:, :], cst[0:96, 1:2], None, ALU.mult, None, t0, [t0, cst])
                k.dma("sp", sin_d[:, :], t0[:, :], reads=[t0])
            else:
                k.dma("sp", cos_d[:, :], t0[:, :], reads=[t0])
        k.barrier()
    if debug == "0":
        return k, st

    with contextlib.ExitStack() as ph:
        k.new_phase()
        NCA = 2208
        Win = k.sb(ph, [128, 8, NCA], BF16, "Win")
        WkrE = k.sb(ph, [128, 8, 96], BF16, "WkrE")
        WkrS = k.sb(ph, [128, 8, 96], BF16, "WkrS")
        WqP = k.sb(ph, [128, 3, 768], BF16, "WqP")
        WqS = k.sb(ph, [128, 3, 768], BF16, "WqS")
        WknE = k.sb(ph, [128, 2, 768], BF16, "WknE")
        Wv = k.sb(ph, [128, 2, 512], BF16, "Wv")
        wsub = contextlib.ExitStack()
        gqT = k.sb(wsub, [128, 3], F32, "gqT")
        gkvT = k.sb(wsub, [128, 2], F32, "gkvT")
        wst = k.sb(wsub, [128, 3, 1024], F32, "wst")
        wvin = w_in_d.rearrange("(c p) n -> p c n", p=128)
        for c in range(8):
            k.dma("pool", Win[:, c, :], wvin[:, c, 0:NCA], writes=[Win])
        k.memset("dve", WkrE[:, :, :], 0.0, WkrE)
        k.memset("dve", WkrS[:, :, :], 0.0, WkrS)
        k.cp("dve", WkrE[:, :, 64:96], Win[:, :, C_KR:C_KR + 32], WkrE, [Win])
        k.cp("dve", WkrS[:, :, 64:80], Win[:, :, C_KR + 16:C_KR + 32], WkrS, [Win])
        k.cp("dve", WkrS[:, :, 80:96], Win[:, :, C_KR:C_KR + 16], WkrS, [Win])
        k.dma("sp", gqT[:, :], gq_d.rearrange("o (c p) -> p (o c)", p=128), writes=[gqT])
        k.dma("sp", gkvT[:, :], gkv_d.rearrange("o (c p) -> p (o c)", p=128), writes=[gkvT])
        k.dma("sp", wst[:, :, 0:768], w_qup_d.rearrange("(c p) n -> p c n", p=128), writes=[wst])
        for c in range(3):
            k.ts(WqP[:, c, :], wst[:, c, 0:768], gqT[:, c:c + 1], None, ALU.mult, None, WqP, [wst, gqT])
        k.memset("pool", WqS[:, :, :], 0.0, WqS)
        wqp4 = WqP.t.rearrange("p c (h e) -> p c h e", e=96)
        wqs4 = WqS.t.rearrange("p c (h e) -> p c h e", e=96)
        for c in range(3):
            k.cp("pool", wqs4[:, c, :, 64:80], wqp4[:, c, :, 80:96], WqS, [WqP])
            k.cp("pool", wqs4[:, c, :, 80:96], wqp4[:, c, :, 64:80], WqS, [WqP])
        k.dma("sp", wst[:, 0:2, :], w_kvup_d.rearrange("(c p) n -> p c n", p=128), writes=[wst])
        k.memset("pool", WknE[:, :, :], 0.0, WknE)
        wkn4 = WknE.t.rearrange("p c (h e) -> p c h e", e=96)
        wv4 = Wv.t.rearrange("p c (h e) -> p c h e", e=64)
        for c in range(2):
            w4 = wst.t[:, c, :].rearrange("p (h e) -> p h e", e=128)
            k.ts(wkn4[:, c, :, 0:64], w4[:, :, 0:64], gkvT[:, c:c + 1], None, ALU.mult, None, WknE, [wst, gkvT])
            k.ts(wv4[:, c, :, :], w4[:, :, 64:128], gkvT[:, c:c + 1], None, ALU.mult, None, Wv, [wst, gkvT])

        k.barrier()
        wsub.close()
        xg = [k.sb(ph, [128, 4, D], F32, "xg") for _ in range(2)]
        uT = [k.sb(ph, [128, 8, 512], BF16, "uT") for _ in range(2)]
        qk_out = [k.sb(ph, [128, 8, 512], BF16, "qk_out") for _ in range(2)]
        vs_out = [k.sb(ph, [128, 4, 512], BF16, "vs_out") for _ in range(2)]
        latT = k.sb(ph, [128, 5, 512], BF16, "latT")
        sq = k.sb(ph, [128, 5, 512], BF16, "sq")
        cosg = [k.sb(ph, [96, 512], F32, "cosg") for _ in range(2)]
        sing = [k.sb(ph, [96, 512], F32, "sing") for _ in range(2)]
        krope = k.sb(ph, [96, 512], F32, "krope")
        sqrq = k.sb(ph, [128, 512], F32, "sqrq")
        sqrkv = k.sb(ph, [128, 512], F32, "sqrkv")
        rq = k.sb(ph, [128, 512], F32, "rq")
        rkv = k.sb(ph, [128, 512], F32, "rkv")
        cosR = k.sb(ph, [96, 512], F32, "cosR")
        sinR = k.sb(ph, [96, 512], F32, "sinR")
        tA = [k.sb(ph, [96, 512], F32, "tA") for _ in range(2)]
        tB = [k.sb(ph, [96, 512], F32, "tB") for _ in range(2)]
        tC = [k.sb(ph, [96, 512], F32, "tC") for _ in range(2)]
        qm_out = [k.sb(ph, [96, 8, 512], BF16, "qm_out") for _ in range(1)]
        km_out = [k.sb(ph, [96, 8, 512], BF16, "km_out") for _ in range(1)]
        vm_out = [k.sb(ph, [128, 4, 640], BF16, "vm_out") for _ in range(2)]
        cols = [k.sb(ph, [128, 2], F32, "cols") for _ in range(2)]
        banks = [k.ps(ph, [128, 512], F32, "bk") for _ in range(8)]
        nb_ = _rr(banks)
        for v in vm_out:
            k.memset("pool", v[:, :, :], 1.0, v)
        xv = x_d.rearrange("(t p) d -> p t d", p=128)
        k.dma("sp", xg[0][:, :, :], xv[:, 0:4, :], writes=[xg[0]])
        for g in range(NG):
            X = xg[g % 2]
            U = uT[g % 2]
            if g + 1 < NG:
                k.dma("sp", xg[(g + 1) % 2][:, :, :], xv[:, 4 * (g + 1):4 * (g + 2), :], writes=[xg[(g + 1) % 2]])
            cg, sg = cosg[g % 2], sing[g % 2]
            k.dma("sp", cg[:, :], cos_d[:, g * 512:(g + 1) * 512], writes=[cg])
            k.dma("sp", sg[:, :], sin_d[:, g * 512:(g + 1) * 512], writes=[sg])
            for c in range(8):
                b = nb_()
                for t in range(4):
                    k.tr(b[:, t * 128:(t + 1) * 128], X[:, t, c * 128:(c + 1) * 128], identF[:, :], b, [X, identF],
                         signal=(t == 3))
                k.act(U[:, c, :], b[:, :], AF.Identity, U, [b, sc1p, modT], scale=sc1p[:, c:c + 1], bias=modT[:, c:c + 1])
            QK = qk_out[g % 2]
            for blk in range(8):
                b = nb_()
                k.mm(b[:, :], [(Win[:, c, blk * 128:(blk + 1) * 128], U[:, c, :]) for c in range(8)], b, [Win, U])
                k.cp("act" if blk % 2 == 0 else "dve", QK[:, blk, :], b[:, :], QK, [b])
            k.dma("sp", qsb_d[:, :, g * 512:(g + 1) * 512].rearrange("a p t -> p a t"), QK[:, 0:4, :], reads=[QK])
            k.dma("sp", ksb_d[:, :, g * 512:(g + 1) * 512].rearrange("a p t -> p a t"), QK[:, 4:8, :], reads=[QK])
            VS = vs_out[g % 2]
            for t in range(4):
                b = nb_()
                k.mm(b[:, :], [(U[:, c, t * 128:(t + 1) * 128], Win[:, c, C_VSB:C_VSB + 512]) for c in range(8)], b, [Win, U])
                k.cp("dve", VS[:, t, :], b[:, :], VS, [b])
            k.dma("sp", vsb_d[g * 512:(g + 1) * 512, :].rearrange("(t p) n -> p t n", p=128), VS[:, :, :], reads=[VS])
            for i in range(5):
                b = nb_()
                c0 = C_QD + i * 128
                k.mm(b[:, :], [(Win[:, c, c0:c0 + 128], U[:, c, :]) for c in range(8)], b, [Win, U])
                k.cp("act", latT[:, i, :], b[:, :], latT, [b])
                k.act(sq[:, i, :], b[:, :], AF.Square, sq, [b])
            b1 = nb_()
            k.mm(b1[0:96, :], [(WkrE[:, c, :], U[:, c, :]) for c in range(8)], b1, [WkrE, U])
            b2 = nb_()
            k.mm(b2[0:96, :], [(WkrS[:, c, :], U[:, c, :]) for c in range(8)], b2, [WkrS, U])
            k.tt("dve", tA[0][:, :], b1[0:96, :], cg[:, :], ALU.mult, tA[0], [b1, cg])
            k.tt("dve", tB[0][:, :], b2[0:96, :], sg[:, :], ALU.mult, tB[0], [b2, sg])
            k.tt("pool", krope[:, :], tA[0][:, :], tB[0][:, :], ALU.add, krope, [tA[0], tB[0]])
            bq = nb_()
            k.mm(bq[:, :], [(onesB[:, :], sq[:, i, :]) for i in range(3)], bq, [onesB, sq])
            k.act(sqrq[:, :], bq[:, :], AF.Sqrt, sqrq, [bq], scale=1.0 / 384.0, bias=1e-6)
            k.op("dve", lambda v: v.reciprocal(rq[:, :], sqrq[:, :]), reads=[sqrq], writes=[rq])
            bkv = nb_()
            k.mm(bkv[:, :], [(onesB[:, :], sq[:, 3 + i, :]) for i in range(2)], bkv, [onesB, sq])
            k.act(sqrkv[:, :], bkv[:, :], AF.Sqrt, sqrkv, [bkv], scale=1.0 / 256.0, bias=1e-6)
            k.op("dve", lambda v: v.reciprocal(rkv[:, :], sqrkv[:, :]), reads=[sqrkv], writes=[rkv])
            k.tt("pool", cosR[:, :], cg[:, :], rq[0:96, :], ALU.mult, cosR, [cg, rq])
            k.tt("pool", sinR[:, :], sg[:, :], rq[0:96, :], ALU.mult, sinR, [sg, rq])
            VM = vm_out[g % 2]
            vm4 = VM.t.rearrange("p t (h e) -> p t h e", e=80)
            for t in range(4):
                co = cols[t % 2]
                b = nb_()
                k.mm(b[:, 0:1], [(sq[:, 3 + i, t * 128:(t + 1) * 128], onesB[:, 0:1]) for i in range(2)], b, [sq, onesB])
                k.act(co[:, 0:1], b[:, 0:1], AF.Sqrt, co, [b], scale=1.0 / 256.0, bias=1e-6)
                k.op("dve", lambda v, co=co: v.reciprocal(co[:, 1:2], co[:, 0:1]), reads=[co], writes=[co])
                b = nb_()
                k.mm(b[:, :], [(latT[:, 3 + i, t * 128:(t + 1) * 128], Wv[:, i, :]) for i in range(2)], b, [latT, Wv])
                k.ts(vm4[:, t, :, 0:64], b.t[:, :].rearrange("p (h e) -> p h e", e=64), co[:, 1:2], None, ALU.mult, None,
                     VM, [b, co])
            k.dma("sp", vm_d[g * 512:(g + 1) * 512, :].rearrange("(t p) n -> p t n", p=128), VM[:, :, :], reads=[VM])
            QM = qm_out[0]
            KM = km_out[0]
            for h in range(8):
                a, bb_, cc = tA[h % 2], tB[h % 2], tC[h % 2]
                b1 = nb_()
                k.mm(b1[0:96, :], [(WqP[:, i, h * 96:(h + 1) * 96], latT[:, i, :]) for i in range(3)], b1, [WqP, latT])
                b2 = nb_()
                k.mm(b2[0:96, :], [(WqS[:, i, h * 96:(h + 1) * 96], latT[:, i, :]) for i in range(3)], b2, [WqS, latT])
                k.tt("dve", a[:, :], b1[0:96, :], cosR[:, :], ALU.mult, a, [b1, cosR])
                k.tt("dve", bb_[:, :], b2[0:96, :], sinR[:, :], ALU.mult, bb_, [b2, sinR])
                k.tt("pool", QM[:, h, :], a[:, :], bb_[:, :], ALU.add, QM, [a, bb_])
                b3 = nb_()
                k.mm(b3[0:96, :], [(WknE[:, i, h * 96:(h + 1) * 96], latT[:, 3 + i, :]) for i in range(2)], b3, [WknE, latT])
                k.tt("dve", cc[:, :], b3[0:96, :], rkv[0:96, :], ALU.mult, cc, [b3, rkv])
                k.tt("pool", KM[:, h, :], cc[:, :], krope[:, :], ALU.add, KM, [cc, krope])
            k.dma("sp", qm_d[:, :, g * 512:(g + 1) * 512].rearrange("h p t -> p h t"), QM[:, :, :], reads=[QM])
            k.dma("sp", km_d[:, :, g * 512:(g + 1) * 512].rearrange("h p t -> p h t"), KM[:, :, :], reads=[KM])
        k.barrier()
    if debug == "A":
        return k, st

    with contextlib.ExitStack() as ph:
        k.new_phase()
        kT = [k.sb(ph, [128, S], BF16, "kTsb") for _ in range(4)]
        Vs = [k.sb(ph, [128, 8, 512], BF16, "Vsb") for _ in range(4)]
        for p in range(4):
            k.dma("sp", kT[p][:, :], ksb_d[p, :, :], writes=[kT[p]])
        vv = vsb_d.rearrange("(t p) n -> p t n", p=128)
        for i in range(4):
            k.dma("sp", Vs[i][:, :, :], vv[:, 8 * i:8 * i + 8, :], writes=[Vs[i]])
        onesF = k.sb(ph, [128, 1024], F32, "onesF")
        maskS = k.sb(ph, [128, 128], BF16, "maskS")
        mtmp = k.sb(ph, [128, 128], F32, "mtmp")
        k.memset("dve", onesF[:, :], 1e-30, onesF)
        k.memset("pool", mtmp[:, :], 0.0, mtmp)
        k.op("pool", lambda g_: g_.affine_select(out=mtmp[:, :], in_=mtmp[:, :], pattern=[[1, 128]], compare_op=ALU.is_gt,
                                                 fill=NEG_BIG, base=0, channel_multiplier=-1), reads=(), writes=[mtmp])
        k.cp("dve", maskS[:, :], mtmp[:, :], maskS, [mtmp])
        qg = [k.sb(ph, [128, 4, 512], BF16, "qg") for _ in range(2)]
        om = [k.sb(ph, [128, 1024], F32, "om") for _ in range(3)]
        cpb = [k.sb(ph, [128, 1032], F32, "cpb") for _ in range(3)]
        wb = [k.sb(ph, [128, 1024], BF16, "wb") for _ in range(3)]
        wT = [k.sb(ph, [128, 1024], BF16, "wT") for _ in range(3)]
        oout = [k.sb(ph, [128, 4, 512], BF16, "oout") for _ in range(2)]
        zb = [k.ps(ph, [128, 1024], F32, "zb") for _ in range(2)]
        wTp = [k.ps(ph, [128, 1024], BF16, "wTp") for _ in range(2)]
        oslots = [k.ps(ph, [128, 64], F32, "oslot") for i in range(2)]
        chunks = []
        for g in range(NG):
            for h in range(8):
                for j in range(4):
                    qb = 4 * g + j
                    nkb = 32 - qb
                    nch = (nkb + 7) // 8
                    for ci in range(nch):
                        kb0 = qb + 8 * ci
                        nb = min(8, 32 - kb0)
                        chunks.append((g, h, j, ci, nch, kb0, nb))
        if LIMIT is not None:
            chunks = chunks[START:LIMIT]
        N = len(chunks)
        slot_of = {}
        sl = [0]

        def S1(n):
            g, h, j, ci, nch, kb0, nb = chunks[n]
            p, a = h // 2, h % 2
            W = nb * 128
            Q = qg[g % 2]
            if (h == 0 and j == 0 and ci == 0) or n == 0:
                k.dma("sp", Q[:, :, :], qsb_d[:, :, g * 512:(g + 1) * 512].rearrange("a p t -> p a t"), writes=[Q])
            z = zb[n % 2]
            lq = Q[a * 64:(a + 1) * 64, p, j * 128:(j + 1) * 128]
            kt = kT[p]
            if ci == 0:
                k.op("pe", lambda pe: pe.matmul(z[:, 0:128], lhsT=lq, rhs=kt[a * 64:(a + 1) * 64, kb0 * 128:kb0 * 128 + 128],
                                                 start=True, stop=False), reads=[Q, kt], writes=[z], signal=False)
                k.op("pe", lambda pe: pe.matmul(z[:, 0:128], lhsT=identB[:, :], rhs=maskS[:, :], start=False, stop=True),
                     reads=[identB, maskS], writes=[], signal=(W == 128))
                segs = [(c0, min(c0 + 512 - (c0 % 512), W)) for c0 in ([128] if W > 128 else []) + ([512] if W > 512 else [])]
                for si_, (c0, c1) in enumerate(segs):
                    k.op("pe", lambda pe, c0=c0, c1=c1: pe.matmul(z[:, c0:c1], lhsT=lq,
                                                                 rhs=kt[a * 64:(a + 1) * 64, kb0 * 128 + c0:kb0 * 128 + c1],
                                                                 start=True, stop=True), reads=[], writes=[], signal=(si_ == len(segs) - 1))
            else:
                segs = [(0, min(512, W))] + ([(512, W)] if W > 512 else [])
                for si_, (c0, c1) in enumerate(segs):
                    k.op("pe", lambda pe, c0=c0, c1=c1: pe.matmul(z[:, c0:c1], lhsT=lq,
                                                                 rhs=kt[a * 64:(a + 1) * 64, kb0 * 128 + c0:kb0 * 128 + c1],
                                                                 start=True, stop=True),
                         reads=[Q, kt] if si_ == 0 else [], writes=[z] if si_ == 0 else [], signal=(si_ == len(segs) - 1))
            o_ = om[n % 3]
            k.act(o_[:, 0:W], z[:, 0:W], AF.Sigmoid, o_, [z], scale=-0.125)
            c_ = cpb[n % 3]
            if ci == 0:
                k.memset("dve", c_[:, 0:1], 1.0, c_)
            else:
                cprev = cpb[(n - 1) % 3]
                k.cp("dve", c_[:, 0:1], cprev[:, 1024:1025], c_, [cprev])
            k.op("dve", lambda v: v.tensor_tensor_scan(out=c_[:, 1:1 + W], data0=o_[:, 0:W], data1=onesF[:, 0:W],
                                                       initial=c_[:, 0:1], op0=ALU.mult, op1=ALU.max),
                 reads=[o_, onesF], writes=[c_])
            w_ = wb[n % 3]
            k.tt("pool", w_[:, 0:W], c_[:, 0:W], c_[:, 1:1 + W], ALU.subtract, w_, [c_])

        def S2(n):
            g, h, j, ci, nch, kb0, nb = chunks[n]
            W = nb * 128
            w_ = wb[n % 3]
            tp = wTp[n % 2]
            for i in range(nb):
                k.tr(tp[:, i * 128:(i + 1) * 128], w_[:, i * 128:(i + 1) * 128], identB[:, :], tp, [w_, identB],
                     signal=(i == nb - 1))
            k.cp("act" if n % 2 == 0 else "dve", wT[n % 3][:, 0:W], tp[:, 0:W], wT[n % 3], [tp])

        def S3(n):
            g, h, j, ci, nch, kb0, nb = chunks[n]
            if ci == 0:
                slot_of[(g, h, j)] = sl[0] % 2
                sl[0] += 1
            si = slot_of[(g, h, j)]
            osl = oslots[si]
            oap = osl[:, :]
            t_ = wT[n % 3]
            for i in range(nb):
                kb = kb0 + i
                vb = Vs[kb // 8]
                first = (ci == 0 and i == 0)
                last = (ci == nch - 1 and i == nb - 1)
                k.op("pe", lambda pe, i=i, kb=kb, vb=vb, first=first, last=last:
                     pe.matmul(oap, lhsT=t_[:, i * 128:(i + 1) * 128], rhs=vb[:, kb % 8, h * 64:(h + 1) * 64],
                               start=first, stop=last),
                     reads=[t_, vb] if i == 0 else [vb], writes=[osl] if i == 0 else [], signal=(i == nb - 1))
            if ci == nch - 1:
                O = oout[g % 2]
                k.cp("dve", O[:, j, h * 64:(h + 1) * 64], oap, O, [osl])
                if h == 7 and j == 3:
                    for tt_ in range(4):
                        r0 = g * 512 + tt_ * 128
                        k.dma("sp", o_d[r0:r0 + 128, 0:512], O[:, tt_, :], reads=[O])

        for n in range(N + SK3):
            if n < N:
                S1(n)
            if 0 <= n - SK2 < N:
                S2(n - SK2)
            if 0 <= n - SK3 < N:
                S3(n - SK3)
        for _ in range(PAD):
            k.memset("dve", mtmp[:, 0:8], 0.0, mtmp)
        k.barrier()
    if debug == "B1s":
        return k, st

    with contextlib.ExitStack() as ph:
        k.new_phase()
        kM = [k.sb(ph, [96, S], BF16, "kM") for _ in range(8)]
        Vm = [k.sb(ph, [128, 8, 640], BF16, "Vm") for _ in range(4)]
        for h in range(8):
            k.dma("sp", kM[h][:, :], km_d[h, :, :], writes=[kM[h]])
        vv = vm_d.rearrange("(t p) n -> p t n", p=128)
        for i in range(4):
            k.dma("sp", Vm[i][:, :, :], vv[:, 8 * i:8 * i + 8, :], writes=[Vm[i]])
        maskM = k.sb(ph, [128, 128], BF16, "maskM")
        zB = k.sb(ph, [128, 128], BF16, "zB")
        k.memset("dve", zB[:, :], 0.0, zB)
        mtmp = k.sb(ph, [128, 128], F32, "mtmp2")
        k.memset("pool", mtmp[:, :], 1.0, mtmp)
        k.op("pool", lambda g_: g_.affine_select(out=mtmp[:, :], in_=mtmp[:, :], pattern=[[-1, 128]], compare_op=ALU.is_gt,
                                                 fill=0.0, base=1, channel_multiplier=1), reads=(), writes=[mtmp])
        k.cp("dve", maskM[:, :], mtmp[:, :], maskM, [mtmp])
        qm = [k.sb(ph, [96, 8, 512], BF16, "qm") for _ in range(2)]
        pT = [k.sb(ph, [128, 512], BF16, "pT") for _ in range(4)]
        oout = [k.sb(ph, [128, 4, 512], BF16, "ooutm") for _ in range(2)]
        rc = [k.sb(ph, [128, 8], F32, "rc") for _ in range(2)]
        sb_ = [k.ps(ph, [128, 512], F32, "sTb") for _ in range(3)]
        oM = [k.ps(ph, [128, 512], F32, "oM") for _ in range(2)]
        SC = 1.0 / (96.0 ** 0.5)
        blocks = []
        for g in range(NG):
            for h in range(8):
                for kb in range(4 * g, 32):
                    blocks.append((g, h, kb))
        if MLIMIT is not None:
            blocks = blocks[:MLIMIT]
        N = len(blocks)

        def M1(n):
            g, h, kb = blocks[n]
            m = kb - 4 * g
            Q = qm[g % 2]
            if h == 0 and m == 0:
                k.dma("sp", Q[:, :, :], qm_d[:, :, g * 512:(g + 1) * 512].rearrange("h p t -> p h t"), writes=[Q])
            s_ = sb_[n % 3]
            kk = kM[h]
            lk = kk[:, kb * 128:(kb + 1) * 128]
            if m >= 4:
                ncol = 512
                k.op("pe", lambda pe: pe.matmul(s_[:, 0:512], lhsT=lk, rhs=Q[:, h, :], start=True, stop=True),
                     reads=[kk, Q], writes=[s_], signal=True)
            else:
                ncol = (m + 1) * 128
                if m > 0:
                    k.op("pe", lambda pe: pe.matmul(s_[:, 0:m * 128], lhsT=lk, rhs=Q[:, h, 0:m * 128], start=True, stop=True),
                         reads=[kk, Q], writes=[s_], signal=False)
                k.op("pe", lambda pe: pe.matmul(s_[:, m * 128:ncol], lhsT=lk, rhs=Q[:, h, m * 128:ncol], start=True, stop=True),
                     reads=[kk, Q], writes=[s_], signal=True)
            k.act(pT[n % 4][:, 0:ncol], s_[:, 0:ncol], AF.Exp, pT[n % 4], [s_], scale=SC)
            if m < 4:
                p__ = pT[n % 4]
                k.tt("dve", p__[:, m * 128:ncol], p__[:, m * 128:ncol], maskM[:, :], ALU.mult, p__, [p__, maskM])

        def M2(n):
            g, h, kb = blocks[n]
            m = kb - 4 * g
            acc = oM[(g * 8 + h) % 2]
            acc4 = acc.t[:, 0:320].rearrange("p (j e) -> p j e", e=80)
            p_ = pT[n % 4]
            vb = Vm[kb // 8]
            vap = vb.t[:, kb % 8, :].rearrange("p (hh e) -> p hh e", e=80)
            nj = min(m, 3) + 1
            if m == 0:
                k.op("pe", lambda pe: pe.matmul(acc.t[:, 0:320], lhsT=zB[:, :], rhs=Vm[0].t[:, 0, 0:320], start=True, stop=False),
                     reads=[zB, Vm[0]], writes=[acc], signal=False)
            for j in range(nj):
                first = False
                last = (kb == 31)
                k.op("pe", lambda pe, j=j, first=first, last=last:
                     pe.matmul(acc4[:, j, 0:66], lhsT=p_[:, j * 128:(j + 1) * 128], rhs=vap[:, h, 0:66], start=first, stop=last),
                     reads=[p_, vb] if j == 0 else [], writes=[acc] if j == 0 else [], signal=(j == nj - 1))
            if kb == 31 and not NOFIN:
                O = oout[g % 2]
                r_ = rc[(g * 8 + h) % 2]
                for j in range(4):
                    k.cp("dve", r_[:, 4 + j:5 + j], acc4[:, j, 64:65], r_, [acc])
                if not DBGM:
                    k.op("dve", lambda v: v.reciprocal(r_[:, 0:4], r_[:, 4:8]), reads=[r_], writes=[r_])
                for j in range(4):
                    if DBGM:
                        k.cp("dve", O[:, j, h * 64:(h + 1) * 64], acc4[:, j, 1:65], O, [acc])
                    else:
                        k.ts(O[:, j, h * 64:(h + 1) * 64], acc4[:, j, 0:64], r_[:, j:j + 1], None, ALU.mult, None, O, [acc, r_])
                if DBGM and MLIMIT is not None and h == 1:
                    for tt_ in range(4):
                        r0 = g * 512 + tt_ * 128
                        k.dma("sp", o_d[r0:r0 + 128, 512:1024], O[:, tt_, :], reads=[O])
                if h == 7:
                    for tt_ in range(4):
                        r0 = g * 512 + tt_ * 128
                        k.dma("sp", o_d[r0:r0 + 128, 512:1024], O[:, tt_, :], reads=[O])

        for n in range(N + 2):
            if n < N:
                M1(n)
            if 0 <= n - 2 < N:
                M2(n - 2)
        k.barrier()
    if debug == "B1":
        return k, st

    with contextlib.ExitStack() as ph:
        k.new_phase()
        wvin = w_in_d.rearrange("(c p) n -> p c n", p=128)
        Wgi = k.sb(ph, [128, 8, 2048], BF16, "Wgi")
        for c in range(8):
            k.dma("pool", Wgi[:, c, :], wvin[:, c, C_GSB:C_GSB + 2048], writes=[Wgi])
        Wb = k.sb(ph, [128, 8, D], BF16, "Wb")
        k.dma("pool", Wb[:, 0:4, :], w_bsb_d.rearrange("(c p) n -> p c n", p=128), writes=[Wb])
        k.dma("pool", Wb[:, 4:8, :], w_bmla_d.rearrange("(c p) n -> p c n", p=128), writes=[Wb])
        Wo = k.sb(ph, [128, 8, D], BF16, "Wo")
        k.dma("pool", Wo[:, :, :], w_out_d.rearrange("(c p) n -> p c n", p=128), writes=[Wo])
        Wr = k.sb(ph, [128, 8, 36], F32, "Wr")
        k.dma("sp", Wr[:, :, 0:4], w_rg_d.rearrange("(c p) n -> p c n", p=128), writes=[Wr])
        k.dma("sp", Wr[:, :, 4:36], w_re_d.rearrange("(c p) n -> p c n", p=128), writes=[Wr])
        brow = k.sb(ph, [128, 36], F32, "brow")
        k.dma("sp", brow[:, 0:4], b_rg_d[0:1, :].partition_broadcast(128), writes=[brow])
        k.dma("sp", brow[:, 4:36], b_re_d[0:1, :].partition_broadcast(128), writes=[brow])
        g1bc = k.sb(ph, [128, D], F32, "g1bc")
        k.dma("sp", g1bc[:, :], mod_d[0:1, 2 * D:3 * D].partition_broadcast(128), writes=[g1bc])
        gA = k.sb(ph, [128, D], F32, "gA")
        bA = k.sb(ph, [128, D], F32, "bA")
        k.dma("sp", gA[:, :], ln1g_d[0:1, :].partition_broadcast(128), writes=[gA])
        k.dma("sp", bA[:, :], ln1b_d[0:1, :].partition_broadcast(128), writes=[bA])
        k.ts(gA[:, :], gA[:, :], ALPHA, None, ALU.mult, None, gA, [gA])
        k.ts(bA[:, :], bA[:, :], ALPHA, None, ALU.mult, None, bA, [bA])
        lgT = k.sb(ph, [128, 8], F32, "lgT")
        lbT = k.sb(ph, [128, 8], F32, "lbT")
        A2 = k.sb(ph, [128, 8], F32, "A2")
        B2 = k.sb(ph, [128, 8], F32, "B2")
        k.dma("sp", lgT[:, :], ln1g_d.rearrange("o (c p) -> p (o c)", p=128), writes=[lgT])
        k.dma("sp", lbT[:, :], ln1b_d.rearrange("o (c p) -> p (o c)", p=128), writes=[lbT])
        k.ts(A2[:, :], modT[:, 32:40], 1.0, None, ALU.add, None, A2, [modT])
        k.tt("dve", B2[:, :], lbT[:, :], A2[:, :], ALU.mult, B2, [lbT, A2])
        k.tt("dve", B2[:, :], B2[:, :], modT[:, 24:32], ALU.add, B2, [B2, modT])
        k.tt("dve", A2[:, :], A2[:, :], lgT[:, :], ALU.mult, A2, [A2, lgT])

        xg = k.sb(ph, [128, 4, D], F32, "xg2")
        og = k.sb(ph, [128, 4, D], BF16, "og")
        uT = k.sb(ph, [128, 8, 512], BF16, "uT2")
        gates = k.sb(ph, [128, 4, 2048], BF16, "gates")
        oT = [k.sb(ph, [128, D], BF16, "oT") for _ in range(2)]
        t1 = k.sb(ph, [128, D], F32, "t1")
        t2 = k.sb(ph, [128, D], F32, "t2")
        mixed = k.sb(ph, [128, D], BF16, "mixed")
        mT = [k.sb(ph, [128, D], BF16, "mT") for _ in range(2)]
        pre = k.sb(ph, [128, 4, D], F32, "pre")
        acc0 = [k.sb(ph, [128, D], F32, "acc0") for _ in range(2)]
        u2T = k.sb(ph, [128, 8, 512], F32, "u2T")
        u2Tb = k.sb(ph, [128, 8, 512], BF16, "u2Tb")
        stt = k.sb(ph, [128, 4, 12], F32, "stt")
        mv = k.sb(ph, [128, 4, 2], F32, "mv")
        sd = k.sb(ph, [128, 4, 2], F32, "sd")
        comb = k.sb(ph, [128, 4, 32], F32, "comb")
        rs = [k.sb(ph, [128, 160], F32, "rs") for _ in range(2)]
        fb = [k.ps(ph, [128, 512], F32, "fb") for _ in range(2)]
        tpb = [k.ps(ph, [128, D], BF16, "tpb") for _ in range(2)]
        yp = [k.ps(ph, [128, D], F32, "yp") for _ in range(2)]
        nfb = _rr(fb)
        ntp = _rr(tpb)
        xv = x_d.rearrange("(t p) d -> p t d", p=128)
        ov = o_d.rearrange("(t p) d -> p t d", p=128)
        for G in range(NG):
            k.dma("sp", xg[:, :, :], xv[:, 4 * G:4 * G + 4, :], writes=[xg])
            k.dma("sp", og[:, :, :], ov[:, 4 * G:4 * G + 4, :], writes=[og])
            for c in range(8):
                b = nfb()
                for t in range(4):
                    k.tr(b[:, t * 128:(t + 1) * 128], xg[:, t, c * 128:(c + 1) * 128], identF[:, :], b, [xg, identF], signal=(t == 3))
                k.act(uT[:, c, :], b[:, :], AF.Identity, uT, [b, sc1p, modT], scale=sc1p[:, c:c + 1], bias=modT[:, c:c + 1])
            for t in range(4):
                for blk in range(4):
                    b = nfb()
                    k.mm(b[:, :], [(uT[:, c, t * 128:(t + 1) * 128], Wgi[:, c, blk * 512:(blk + 1) * 512]) for c in range(8)], b, [uT, Wgi])
                    k.act(gates[:, t, blk * 512:(blk + 1) * 512], b[:, :], AF.Sigmoid, gates, [b])
            for t in range(4):
                tp = ntp()
                for c in range(8):
                    k.tr(tp[:, c * 128:(c + 1) * 128], og[:, t, c * 128:(c + 1) * 128], identB[:, :], tp, [og, identB], signal=(c == 7))
                o_ = oT[t % 2]
                k.cp("dve", o_[:, :], tp[:, :], o_, [tp])
                ysb, yml = yp[0], yp[1]
                for hf in range(2):
                    k.mm(ysb[:, hf * 512:(hf + 1) * 512], [(o_[:, c * 128:(c + 1) * 128], Wb[:, c, hf * 512:(hf + 1) * 512]) for c in range(4)],
                         ysb, [o_, Wb], signal=(hf == 1))
                for hf in range(2):
                    k.mm(yml[:, hf * 512:(hf + 1) * 512], [(o_[:, c * 128:(c + 1) * 128], Wb[:, c, hf * 512:(hf + 1) * 512]) for c in range(4, 8)],
                         yml, [o_, Wb], signal=(hf == 1))
                k.tt("dve", t1[:, :], ysb[:, :], gates[:, t, 0:D], ALU.mult, t1, [ysb, gates])
                k.tt("dve", t2[:, :], yml[:, :], gates[:, t, D:2 * D], ALU.mult, t2, [yml, gates])
                k.tt("pool", mixed[:, :], t1[:, :], t2[:, :], ALU.add, mixed, [t1, t2])
                tp = ntp()
                for c in range(8):
                    k.tr(tp[:, c * 128:(c + 1) * 128], mixed[:, c * 128:(c + 1) * 128], identB[:, :], tp, [mixed, identB], signal=(c == 7))
                m_ = mT[t % 2]
                k.cp("act", m_[:, :], tp[:, :], m_, [tp])
                at = ysb
                for hf in range(2):
                    k.mm(at[:, hf * 512:(hf + 1) * 512], [(m_[:, c * 128:(c + 1) * 128], Wo[:, c, hf * 512:(hf + 1) * 512]) for c in range(8)],
                         at, [m_, Wo], signal=(hf == 1))
                k.tt("dve", t1[:, :], at[:, :], g1bc[:, :], ALU.mult, t1, [at, g1bc])
                k.op("dve", lambda v, t=t: v.scalar_tensor_tensor(out=pre[:, t, :], in0=xg[:, t, :], scalar=ALPHA, in1=t1[:, :],
                                                                    op0=ALU.mult, op1=ALU.add), reads=[xg, t1], writes=[pre])
                for hf in range(2):
                    k.op("dve", lambda v, t=t, hf=hf: v.bn_stats(out=stt[:, t, hf * 6:(hf + 1) * 6], in_=pre[:, t, hf * 512:(hf + 1) * 512]),
                         reads=[pre], writes=[stt])
                k.op("dve", lambda v, t=t: v.bn_aggr(out=mv[:, t, :], in_=stt[:, t, :].rearrange("p (a b) -> p a b", b=6)),
                     reads=[stt], writes=[mv])
            for t in range(4):
                k.act(sd[:, t, 0:1], mv[:, t, 1:2], AF.Sqrt, sd, [mv], scale=1.0, bias=1e-5)
                k.op("dve", lambda v, t=t: v.reciprocal(sd[:, t, 1:2], sd[:, t, 0:1]), reads=[sd], writes=[sd])
                k.ts(pre[:, t, :], pre[:, t, :], mv[:, t, 0:1], sd[:, t, 1:2], ALU.subtract, ALU.mult, pre, [pre, mv, sd])
                a_ = acc0[t % 2]
                k.tt("pool", a_[:, :], pre[:, t, :], gA[:, :], ALU.mult, a_, [pre, gA])
                k.tt("pool", a_[:, :], a_[:, :], bA[:, :], ALU.add, a_, [a_, bA])
                k.dma("sp", acc_d[(4 * G + t) * 128:(4 * G + t + 1) * 128, :], a_[:, :], reads=[a_])
            for c in range(8):
                b = nfb()
                for t in range(4):
                    k.tr(b[:, t * 128:(t + 1) * 128], pre[:, t, c * 128:(c + 1) * 128], identF[:, :], b, [pre, identF], signal=(t == 3))
                k.act(u2T[:, c, :], b[:, :], AF.Identity, u2T, [b, A2, B2], scale=A2[:, c:c + 1], bias=B2[:, c:c + 1])
            k.cp("pool", u2Tb[:, :, :], u2T[:, :, :], u2Tb, [u2T])
            k.dma("sp", u2T_d[:, :, G * 512:(G + 1) * 512].rearrange("c p t -> p c t"), u2Tb[:, :, :], reads=[u2Tb])
            for t in range(4):
                b = nfb()
                r_ = rs[t % 2]
                k.mm(b[:, 0:36], [(u2T[:, c, t * 128:(t + 1) * 128], Wr[:, c, :]) for c in range(8)], b, [u2T, Wr])
                lg = r_[:, 0:36]
                k.tt("dve", lg, b[:, 0:36], brow[:, :], ALU.add, r_, [b, brow])
                gmax, ngm, sg4, om4, e4, ssum, pg = (r_[:, 40:41], r_[:, 41:42], r_[:, 44:48], r_[:, 48:52], r_[:, 52:56],
                                                     r_[:, 42:43], r_[:, 43:44])
                oh, elm, top8 = r_[:, 56:60], r_[:, 64:96], r_[:, 96:104]
                dd, sgd, w1, w2, c1 = r_[:, 104:105], r_[:, 105:106], r_[:, 106:107], r_[:, 107:108], r_[:, 112:144]
                R = [r_]
                k.op("dve", lambda v: v.tensor_reduce(out=gmax, in_=r_[:, 0:4], axis=AX.X, op=ALU.max), reads=R, writes=R)
                k.ts(ngm, gmax, -1.0, None, ALU.mult, None, r_, R)
                k.act(sg4, r_[:, 0:4], AF.Sigmoid, r_, R, bias=ngm)
                k.ts(om4, sg4, -1.0, 1.0, ALU.mult, ALU.add, r_, R)
                k.op("dve", lambda v: v.reciprocal(om4, om4), reads=R, writes=R)
                k.tt("dve", e4, sg4, om4, ALU.mult, r_, R)
                k.op("dve", lambda v: v.tensor_reduce(out=ssum, in_=e4, axis=AX.X, op=ALU.add), reads=R, writes=R)
                k.op("dve", lambda v: v.reciprocal(pg, ssum), reads=R, writes=R)
                k.ts(oh, r_[:, 0:4], gmax, None, ALU.is_equal, None, r_, R)
                k.ts(oh, oh, 1e30, -1e30, ALU.mult, ALU.add, r_, R)
                for gi in range(4):
                    k.ts(r_[:, 64 + 8 * gi:72 + 8 * gi], r_[:, 4 + 8 * gi:12 + 8 * gi], r_[:, 56 + gi:57 + gi], None, ALU.add, None, r_, R)
                k.op("dve", lambda v: v.max(out=top8, in_=elm), reads=R, writes=R)
                k.tt("dve", dd, r_[:, 96:97], r_[:, 97:98], ALU.subtract, r_, R)
                k.act(sgd, dd, AF.Sigmoid, r_, R)
                k.tt("dve", w1, pg, sgd, ALU.mult, r_, R)
                k.tt("dve", w2, pg, w1, ALU.subtract, r_, R)
                k.ts(c1, elm, r_[:, 96:97], w1, ALU.is_equal, ALU.mult, r_, R)
                k.ts(comb[:, t, :], elm, r_[:, 97:98], w2, ALU.is_equal, ALU.mult, comb, R)
                k.tt("dve", comb[:, t, :], comb[:, t, :], c1, ALU.add, comb, [comb, r_])
            k.dma("sp", comb_d[G * 512:(G + 1) * 512, :].rearrange("(t p) n -> p t n", p=128), comb[:, :, :], reads=[comb])
        k.barrier()
    if debug == "B2":
        return k, st

    with contextlib.ExitStack() as ph:
        k.new_phase()
        g2bc = k.sb(ph, [128, D], F32, "g2bc")
        l2g = k.sb(ph, [128, D], F32, "l2g")
        l2b = k.sb(ph, [128, D], F32, "l2b")
        k.dma("sp", g2bc[:, :], mod_d[0:1, 5 * D:6 * D].partition_broadcast(128), writes=[g2bc])
        k.dma("sp", l2g[:, :], ln2g_d[0:1, :].partition_broadcast(128), writes=[l2g])
        k.dma("sp", l2b[:, :], ln2b_d[0:1, :].partition_broadcast(128), writes=[l2b])
        acc = k.sb(ph, [128, 16, D], F32, "acc")
        u2 = k.sb(ph, [128, 8, 2048], BF16, "u2")
        cmb = k.sb(ph, [128, 16, 32], F32, "cmb")
        Wg = [k.sb(ph, [128, 8, 256], BF16, "Wg") for _ in range(2)]
        Wu = [k.sb(ph, [128, 8, 256], BF16, "Wu") for _ in range(2)]
        Wds = [k.sb(ph, [128, 2, D], F32, "Wds") for _ in range(2)]
        Wd = [k.sb(ph, [128, 2, D], BF16, "Wd") for _ in range(2)]
        sgb = [k.sb(ph, [128, 512], F32, "sgb") for _ in range(2)]
        hT = [k.sb(ph, [128, 2, 512], BF16, "hT") for _ in range(2)]
        stt = k.sb(ph, [128, 12], F32, "stt2")
        mv = k.sb(ph, [128, 16, 2], F32, "mv2")
        sd = k.sb(ph, [128, 16, 2], F32, "sd2")
        yo = [k.sb(ph, [128, D], F32, "yo") for _ in range(2)]
        gub = [k.ps(ph, [128, 512], F32, "gub") for _ in range(4)]
        dnb = [k.ps(ph, [128, D], F32, "dnb") for _ in range(2)]
        ngu = _rr(gub)
        ndn = _rr(dnb)
        NE = 32
        for ps_ in range(2):
            T0 = ps_ * 2048
            k.dma("sp", acc[:, :, :], acc_d[T0:T0 + 2048, :].rearrange("(t p) d -> p t d", p=128), writes=[acc])
            k.dma("sp", u2[:, :, :], u2T_d[:, :, T0:T0 + 2048].rearrange("c p t -> p c t"), writes=[u2])
            k.dma("sp", cmb[:, :, :], comb_d[T0:T0 + 2048, :].rearrange("(t p) n -> p t n", p=128), writes=[cmb])
            pend = None
            seq = [(e, gr) for e in range(NE) for gr in range(4)]

            def load_w(e):
                i = e % 2
                k.dma("pool", Wg[i][:, :, :], w_eg_d[e].rearrange("(c p) f -> p c f", p=128), writes=[Wg[i]])
                k.dma("pool", Wu[i][:, :, :], w_eu_d[e].rearrange("(c p) f -> p c f", p=128), writes=[Wu[i]])
                k.dma("sp", Wds[i][:, :, :], w_ed_d[e].rearrange("(c p) d -> p c d", p=128), writes=[Wds[i]])
                for c in range(2):
                    k.tt("pool", Wd[i][:, c, :], Wds[i][:, c, :], g2bc[:, :], ALU.mult, Wd[i], [Wds[i], g2bc])

            def gate_up(e, gr, it):
                i = e % 2
                H = hT[it % 2]
                for fc in range(2):
                    bg = ngu()
                    k.mm(bg[:, :], [(Wg[i][:, c, fc * 128:(fc + 1) * 128], u2[:, c, gr * 512:(gr + 1) * 512]) for c in range(8)], bg, [Wg[i], u2])
                    bu = ngu()
                    k.mm(bu[:, :], [(Wu[i][:, c, fc * 128:(fc + 1) * 128], u2[:, c, gr * 512:(gr + 1) * 512]) for c in range(8)], bu, [Wu[i], u2])
                    sg_ = sgb[fc]
                    k.act(sg_[:, :], bg[:, :], AF.Silu, sg_, [bg])
                    k.tt("dve", H[:, fc, :], sg_[:, :], bu[:, :], ALU.mult, H, [sg_, bu])

            def down(e, gr, it):
                i = e % 2
                H = hT[it % 2]
                for t in range(4):
                    tile = gr * 4 + t
                    bd = ndn()
                    for hf in range(2):
                        k.mm(bd[:, hf * 512:(hf + 1) * 512], [(H[:, fc, t * 128:(t + 1) * 128], Wd[i][:, fc, hf * 512:(hf + 1) * 512]) for fc in range(2)],
                             bd, [H, Wd[i]], signal=(hf == 1))
                    k.op("dve", lambda v, tile=tile, bd=bd: v.scalar_tensor_tensor(out=acc[:, tile, :], in0=bd[:, :], scalar=cmb[:, tile, e:e + 1],
                                                                                  in1=acc[:, tile, :], op0=ALU.mult, op1=ALU.add),
                         reads=[bd, cmb, acc], writes=[acc])

            load_w(0)
            for it, (e, gr) in enumerate(seq):
                gate_up(e, gr, it)
                if pend is not None:
                    down(*pend)
                pend = (e, gr, it)
                if gr == 0 and e + 1 < NE:
                    load_w(e + 1)
            down(*pend)
            for t in range(16):
                for hf in range(2):
                    k.op("dve", lambda v, t=t, hf=hf: v.bn_stats(out=stt[:, hf * 6:(hf + 1) * 6], in_=acc[:, t, hf * 512:(hf + 1) * 512]),
                         reads=[acc], writes=[stt])
                k.op("dve", lambda v, t=t: v.bn_aggr(out=mv[:, t, :], in_=stt[:, :].rearrange("p (a b) -> p a b", b=6)),
                     reads=[stt], writes=[mv])
            for t in range(16):
                k.act(sd[:, t, 0:1], mv[:, t, 1:2], AF.Sqrt, sd, [mv], scale=1.0, bias=1e-5)
                k.op("dve", lambda v, t=t: v.reciprocal(sd[:, t, 1:2], sd[:, t, 0:1]), reads=[sd], writes=[sd])
                y_ = yo[t % 2]
                k.ts(y_[:, :], acc[:, t, :], mv[:, t, 0:1], sd[:, t, 1:2], ALU.subtract, ALU.mult, y_, [acc, mv, sd])
                k.tt("pool", y_[:, :], y_[:, :], l2g[:, :], ALU.mult, y_, [y_, l2g])
                k.tt("pool", y_[:, :], y_[:, :], l2b[:, :], ALU.add, y_, [y_, l2b])
                k.dma("sp", out_d[T0 + t * 128:T0 + (t + 1) * 128, :], y_[:, :], reads=[y_])
            k.barrier()
    return k, st


def _prep_inputs(inputs):
    f = lambda a: np.ascontiguousarray(np.asarray(a))
    x = f(inputs["x"])[:, ::-1, :]
    pos = f(inputs["positions"])[:, ::-1]
    inv = (1.0 / (10000.0 ** (np.arange(0, 32, 2, dtype=np.float32) / 32.0))).astype(np.float32)
    cst = np.zeros((128, 4), np.float32)
    cst[64:80, 0] = inv
    cst[80:96, 0] = inv
    cst[64:80, 1] = -1.0
    cst[80:96, 1] = 1.0
    shared = {"cst": cst}
    for name in ("w_ada", "b_ada", "w_in", "mla_q_norm_g", "w_q_up", "mla_kv_norm_g", "w_kv_up", "w_branch_sb",
                 "w_branch_mla", "w_out", "ln1_g", "ln1_b", "w_router_group", "b_router_group", "w_router_expert",
                 "b_router_expert", "w_exp_gate", "w_exp_up", "w_exp_down", "ln2_g", "ln2_b"):
        a = f(inputs[name])
        a = a.reshape(a.shape[1:]) if a.ndim >= 3 else a
        shared[name] = np.ascontiguousarray(a)
    maps = []
    for b in range(8):
        m = dict(shared)
        m["x"] = np.ascontiguousarray(x[b])
        m["c"] = np.ascontiguousarray(f(inputs["c"])[b:b + 1])
        m["positions"] = np.ascontiguousarray(pos[b:b + 1]).astype(np.int32)
        maps.append(m)
    return maps


def kernel(**inputs):
    maps = _prep_inputs(inputs)
    k, st = build()
    res = run_bass_kernel_spmd(k.nc, maps, core_ids=list(range(8)))
    out = np.stack([np.asarray(r["out"]) for r in res.results], axis=0)
    return np.ascontiguousarray(out[:, ::-1, :]).astype(np.float32)
```

```python
import contextlib
import numpy as np
import concourse.bass as bass
import concourse.mybir as mybir
from concourse.bass_utils import run_bass_kernel_spmd

F32 = mybir.dt.float32
BF16 = mybir.dt.bfloat16
I32 = mybir.dt.int32
ALU = mybir.AluOpType
AF = mybir.ActivationFunctionType
AX = mybir.AxisListType

S = 4096
D = 1024
NT = 32
NG = 8
C_QSB, C_KSB, C_VSB, C_QD, C_KVD, C_KR, C_GSB, C_GMLA = 0, 512, 1024, 1536, 1920, 2176, 2208, 3232
ALPHA = 2.0 ** 0.25
MAGIC = 12582912.0
TWO_PI = 6.283185307179586
NEG_BIG = -30000.0
SAME_ENGINE_SYNC = True
LIMIT = None
PAD = 0
START = 0
MLIMIT = None
DBGM = False
NOFIN = False
SK2, SK3 = 1, 2


class Buf:
    __slots__ = ("t", "w", "r", "ds", "sw", "name")

    def __init__(self, t, name=""):
        self.t = t
        self.w = None
        self.r = {}
        self.ds = None
        self.sw = None
        self.name = name

    def __getitem__(self, k):
        return self.t[k]


class DSem:
    __slots__ = ("sem", "count")

    def __init__(self, sem):
        self.sem = sem
        self.count = 0


class KB:
    def __init__(self):
        nc = bass.Bass("TRN2", target_bir_lowering=False)
        self.nc = nc
        self.eng = {"pe": nc.tensor, "act": nc.scalar, "dve": nc.vector, "pool": nc.gpsimd, "sp": nc.sync}
        self.root = contextlib.ExitStack()
        self.esem = {}
        self.tick = {}
        for e in ("pe", "act", "dve", "pool"):
            self.esem[e] = self.root.enter_context(nc.semaphore("es_" + e))
            self.tick[e] = 0
        self.waited = {e: {} for e in self.eng}
        self.dpool = [DSem(self.root.enter_context(nc.semaphore("ds%d" % i))) for i in range(80)]
        self.swpool = [DSem(self.root.enter_context(nc.semaphore("sw%d" % i))) for i in range(8)]
        self.swnext = 0
        self.dnext = 0
        self.dbase = None
        self.uid = 0

    def sb(self, st, shape, dt, name=None):
        self.uid += 1
        name = "%s_%d" % (name or "t", self.uid)
        return Buf(st.enter_context(self.nc.sbuf_tensor(name, shape, dt)), name)

    def ps(self, st, shape, dt, name=None):
        self.uid += 1
        name = "%s_%d" % (name or "p", self.uid)
        return Buf(st.enter_context(self.nc.psum_tensor(name, shape, dt)), name)

    def view(self, buf, name=""):
        return Buf(buf.t, name)

    def new_phase(self):
        if self.dbase is None:
            self.dbase = self.dnext
        self.dnext = self.dbase

    def _dsem(self, b):
        if b.ds is None:
            b.ds = self.dpool[self.dnext]
            self.dnext += 1
            if self.dnext >= len(self.dpool):
                self.dnext = self.dbase or 0
        return b.ds

    def _sync(self, e, reads, writes):
        need = {}
        wd = self.waited[e]

        def add(m):
            if m is None:
                return
            sem, val, src = m
            if src == e and (e == "pe" or not SAME_ENGINE_SYNC):
                return
            key = id(sem)
            if wd.get(key, 0) >= val:
                return
            if key not in need or need[key][1] < val:
                need[key] = (sem, val)

        for b in reads:
            add(b.w)
        for b in writes:
            add(b.w)
            for m in b.r.values():
                add(m)
        for key, (sem, val) in need.items():
            self.eng[e].wait_ge(sem, val)
            wd[key] = val

    def op(self, e, fn, reads=(), writes=(), signal=True):
        self._sync(e, reads, writes)
        ins = fn(self.eng[e])
        if signal:
            self.tick[e] += 1
            ins.then_inc(self.esem[e], 1)
            m = (self.esem[e], self.tick[e], e)
        else:
            m = (self.esem[e], self.tick[e] + 1, e)
        k = id(m[0])
        for b in reads:
            b.r[k] = m
        for b in writes:
            b.w = m
            b.r = {}
        return ins

    def dma(self, q, out, in_, reads=(), writes=()):
        self._sync(q, reads, writes)
        ins = self.eng[q].dma_start(out=out, in_=in_)
        owner = writes[0] if writes else reads[0]
        if q == "pool":
            if owner.sw is None:
                owner.sw = self.swpool[self.swnext]
                self.swnext += 1
            ds = owner.sw
        else:
            ds = self._dsem(owner)
        ds.count += 16
        ins.then_inc(ds.sem, 16)
        m = (ds.sem, ds.count, "dma")
        k = id(ds.sem)
        for b in reads:
            b.r[k] = m
        for b in writes:
            b.w = m
            b.r = {}
        return ins

    def barrier(self):
        for e in self.eng:
            for o in ("pe", "act", "dve", "pool"):
                if o != e and self.tick[o] > self.waited[e].get(id(self.esem[o]), 0):
                    self.eng[e].wait_ge(self.esem[o], self.tick[o])
                    self.waited[e][id(self.esem[o])] = self.tick[o]
            for ds in self.dpool + self.swpool:
                if ds.count > self.waited[e].get(id(ds.sem), 0):
                    self.eng[e].wait_ge(ds.sem, ds.count)
                    self.waited[e][id(ds.sem)] = ds.count

    def mm(self, out_ap, pairs, outb, inbs, start=True, stop=True, signal=True):
        n = len(pairs)
        for i, (l, r) in enumerate(pairs):
            st = start and i == 0
            sp = stop and i == n - 1
            self.op("pe", lambda pe, l=l, r=r, st=st, sp=sp: pe.matmul(out_ap, lhsT=l, rhs=r, start=st, stop=sp),
                    reads=inbs if i == 0 else (), writes=[outb] if i == 0 else (),
                    signal=(signal and i == n - 1))
        if n and not signal:
            pass

    def tr(self, out_ap, in_ap, ident_ap, outb, inbs, signal=True):
        self.op("pe", lambda pe: pe.transpose(out_ap, in_ap, ident_ap), reads=inbs, writes=[outb], signal=signal)

    def act(self, out, in_, func, outb, inbs, scale=1.0, bias=None, accum_out=None, extra_w=()):
        kw = {}
        if bias is not None:
            kw["bias"] = bias
        if accum_out is not None:
            kw["accum_out"] = accum_out
        return self.op("act", lambda a: a.activation(out=out, in_=in_, func=func, scale=scale, **kw),
                       reads=inbs, writes=[outb] + list(extra_w))

    def tt(self, e, out, in0, in1, op, outb, inbs):
        return self.op(e, lambda v: v.tensor_tensor(out=out, in0=in0, in1=in1, op=op), reads=inbs, writes=[outb])

    def ts(self, out, in0, s1, s2, op0, op1, outb, inbs):
        if op1 is None:
            return self.op("dve", lambda v: v.tensor_single_scalar(out=out, in_=in0, scalar=s1, op=op0),
                           reads=inbs, writes=[outb])
        return self.op("dve", lambda v: v.tensor_scalar(out=out, in0=in0, scalar1=s1, scalar2=s2, op0=op0, op1=op1),
                       reads=inbs, writes=[outb])

    def cp(self, e, out, in_, outb, inbs):
        if e == "act":
            return self.act(out, in_, AF.Copy, outb, inbs)
        return self.op(e, lambda v: v.tensor_copy(out, in_), reads=inbs, writes=[outb])

    def memset(self, e, ap, val, outb):
        return self.op(e, lambda v: v.memset(ap, val), reads=(), writes=[outb])


def _rr(lst):
    i = [0]

    def nxt():
        b = lst[i[0] % len(lst)]
        i[0] += 1
        return b
    return nxt


def build(debug=None):
    k = KB()
    nc = k.nc
    es = k.root
    dbg = debug is not None
    skind = "ExternalOutput" if dbg else "Internal"

    def din(name, shape, dt=F32):
        return nc.dram_tensor(name, shape, dt, kind="ExternalInput").ap()

    def dsc(name, shape, dt):
        return nc.dram_tensor(name, shape, dt, kind=skind).ap()

    x_d = din("x", [S, D])
    c_d = din("c", [1, D])
    pos_d = din("positions", [1, S], I32)
    cst_d = din("cst", [128, 4])
    w_ada_d = din("w_ada", [D, 6 * D])
    b_ada_d = din("b_ada", [1, 6 * D])
    w_in_d = din("w_in", [D, 4256])
    gq_d = din("mla_q_norm_g", [1, 384])
    w_qup_d = din("w_q_up", [384, 768])
    gkv_d = din("mla_kv_norm_g", [1, 256])
    w_kvup_d = din("w_kv_up", [256, 1024])
    w_bsb_d = din("w_branch_sb", [512, D])
    w_bmla_d = din("w_branch_mla", [512, D])
    w_out_d = din("w_out", [D, D])
    ln1g_d = din("ln1_g", [1, D])
    ln1b_d = din("ln1_b", [1, D])
    w_rg_d = din("w_router_group", [D, 4])
    b_rg_d = din("b_router_group", [1, 4])
    w_re_d = din("w_router_expert", [D, 32])
    b_re_d = din("b_router_expert", [1, 32])
    NE_DECL = 1 if (dbg and debug != "C") else 32
    w_eg_d = din("w_exp_gate", [NE_DECL, D, 256])
    w_eu_d = din("w_exp_up", [NE_DECL, D, 256])
    w_ed_d = din("w_exp_down", [NE_DECL, 256, D])
    ln2g_d = din("ln2_g", [1, D])
    ln2b_d = din("ln2_b", [1, D])
    out_d = nc.dram_tensor("out", [S, D], F32, kind="ExternalOutput").ap()

    mod_d = dsc("mod_s", [1, 6 * D], F32)
    cos_d = dsc("cos_s", [96, S], F32)
    sin_d = dsc("sin_s", [96, S], F32)
    qsb_d = dsc("qsb_s", [4, 128, S], BF16)
    ksb_d = dsc("ksb_s", [4, 128, S], BF16)
    vsb_d = dsc("vsb_s", [S, 512], BF16)
    qm_d = dsc("qm_s", [8, 96, S], BF16)
    km_d = dsc("km_s", [8, 96, S], BF16)
    vm_d = dsc("vm_s", [S, 640], BF16)
    o_d = dsc("o_s", [S, 1024], BF16)
    acc_d = dsc("acc_s", [S, D], F32)
    u2T_d = dsc("u2T_s", [8, 128, S], BF16)
    comb_d = dsc("comb_s", [S, 32], F32)

    st = contextlib.ExitStack()
    st.enter_context(nc.allow_non_contiguous_dma(reason="small strided setup loads"))

    identF = k.sb(es, [128, 128], F32, "identF")
    identB = k.sb(es, [128, 128], BF16, "identB")
    onesB = k.sb(es, [128, 128], BF16, "onesB")
    modT = k.sb(es, [128, 48], F32, "modT")
    sc1p = k.sb(es, [128, 8], F32, "sc1p")
    cst = k.sb(es, [128, 4], F32, "cst")

    k.memset("pool", identF[:, :], 1.0, identF)
    k.op("pool", lambda g: g.affine_select(out=identF[:, :], in_=identF[:, :], pattern=[[-1, 128]],
                                           compare_op=ALU.is_equal, fill=0.0, base=0, channel_multiplier=1),
         reads=(), writes=[identF])
    k.cp("dve", identB[:, :], identF[:, :], identB, [identF])
    k.memset("dve", onesB[:, :], 1.0, onesB)
    k.dma("sp", cst[:, :], cst_d[:, :], writes=[cst])

    with contextlib.ExitStack() as ph:
        k.new_phase()
        cT = k.sb(ph, [128, 8], F32, "cT")
        cA = k.sb(ph, [128, 8], F32, "cA")
        bada = k.sb(ph, [1, 6 * D], F32, "bada")
        modrow = k.sb(ph, [1, 6 * D], F32, "modrow")
        wa = [k.sb(ph, [128, 8, 512], F32, "wa") for _ in range(2)]
        pm = [k.ps(ph, [128, 512], F32, "pm") for _ in range(2)]
        mod_db = Buf(None, "mod_d")
        k.dma("sp", cT[:, :], c_d.rearrange("o (c p) -> p (o c)", p=128), writes=[cT])
        k.dma("sp", bada[:, :], b_ada_d[:, :], writes=[bada])
        k.act(cA[:, :], cT[:, :], AF.Silu, cA, [cT])
        wv = w_ada_d.rearrange("(c p) n -> p c n", p=128)
        for nb in range(12):
            w = wa[nb % 2]
            k.dma("sp", w[:, :, :], wv[:, :, nb * 512:(nb + 1) * 512], writes=[w])
            p = pm[nb % 2]
            k.mm(p[0:1, :], [(cA[:, c:c + 1], w[:, c, :]) for c in range(8)], p, [cA, w])
            k.tt("dve", modrow[0:1, nb * 512:(nb + 1) * 512], p[0:1, :], bada[0:1, nb * 512:(nb + 1) * 512],
                 ALU.add, modrow, [p, bada])
        k.dma("sp", mod_d[:, :], modrow[:, :], reads=[modrow], writes=[mod_db])
        k.dma("sp", modT[:, :], mod_d.rearrange("o (j p) -> p (o j)", p=128), reads=[mod_db], writes=[modT])
        k.ts(sc1p[:, :], modT[:, 8:16], 1.0, None, ALU.add, None, sc1p, [modT])

        posi = k.sb(ph, [96, S], I32, "posi")
        ang = k.sb(ph, [96, S], F32, "ang")
        t0 = k.sb(ph, [96, S], F32, "t0")
        t1 = k.sb(ph, [96, S], F32, "t1")
        k.dma("sp", posi[:, :], pos_d[0:1, :].partition_broadcast(96), writes=[posi])
        k.cp("pool", ang[:, :], posi[:, :], ang, [posi])
        k.ts(ang[:, :], ang[:, :], cst[0:96, 0:1], None, ALU.mult, None, ang, [ang, cst])
        for which in range(2):
            src = ang
            if which == 1:
                k.ts(t1[:, :], ang[:, :], TWO_PI / 4.0, None, ALU.add, None, t1, [ang])
                src = t1
            k.ts(t0[:, :], src[:, :], 1.0 / TWO_PI, MAGIC, ALU.mult, ALU.add, t0, [src])
            k.ts(t0[:, :], t0[:, :], -MAGIC, -TWO_PI, ALU.add, ALU.mult, t0, [t0])
            k.tt("dve", t0[:, :], t0[:, :], src[:, :], ALU.add, t0, [t0, src])
            k.ts(t0[:, :], t0[:, :], -3.1415925, 3.1415925, ALU.max, ALU.min, t0, [t0])
            k.act(t0[:, :], t0[:, :], AF.Sin, t0, [t0])
            if which == 0:
                k.ts(t0[:, :], t0[:, :], cst[0:96, 1:2], None, ALU.mult, None, t0, [t0, cst])
                k.dma("sp", sin_d[:, :], t0[:, :], reads=[t0])
            else:
                k.dma("sp", cos_d[:, :], t0[:, :], reads=[t0])
        k.barrier()
    if debug == "0":
        return k, st

    with contextlib.ExitStack() as ph:
        k.new_phase()
        NCA = 2208
        Win = k.sb(ph, [128, 8, NCA], BF16, "Win")
        WkrE = k.sb(ph, [128, 8, 96], BF16, "WkrE")
        WkrS = k.sb(ph, [128, 8, 96], BF16, "WkrS")
        WqP = k.sb(ph, [128, 3, 768], BF16, "WqP")
        WqS = k.sb(ph, [128, 3, 768], BF16, "WqS")
        WknE = k.sb(ph, [128, 2, 768], BF16, "WknE")
        Wv = k.sb(ph, [128, 2, 512], BF16, "Wv")
        wsub = contextlib.ExitStack()
        gqT = k.sb(wsub, [128, 3], F32, "gqT")
        gkvT = k.sb(wsub, [128, 2], F32, "gkvT")
        wst = k.sb(wsub, [128, 3, 1024], F32, "wst")
        wvin = w_in_d.rearrange("(c p) n -> p c n", p=128)
        for c in range(8):
            k.dma("pool", Win[:, c, :], wvin[:, c, 0:NCA], writes=[Win])
        k.memset("dve", WkrE[:, :, :], 0.0, WkrE)
        k.memset("dve", WkrS[:, :, :], 0.0, WkrS)
        k.cp("dve", WkrE[:, :, 64:96], Win[:, :, C_KR:C_KR + 32], WkrE, [Win])
        k.cp("dve", WkrS[:, :, 64:80], Win[:, :, C_KR + 16:C_KR + 32], WkrS, [Win])
        k.cp("dve", WkrS[:, :, 80:96], Win[:, :, C_KR:C_KR + 16], WkrS, [Win])
        k.dma("sp", gqT[:, :], gq_d.rearrange("o (c p) -> p (o c)", p=128), writes=[gqT])
        k.dma("sp", gkvT[:, :], gkv_d.rearrange("o (c p) -> p (o c)", p=128), writes=[gkvT])
        k.dma("sp", wst[:, :, 0:768], w_qup_d.rearrange("(c p) n -> p c n", p=128), writes=[wst])
        for c in range(3):
            k.ts(WqP[:, c, :], wst[:, c, 0:768], gqT[:, c:c + 1], None, ALU.mult, None, WqP, [wst, gqT])
        k.memset("pool", WqS[:, :, :], 0.0, WqS)
        wqp4 = WqP.t.rearrange("p c (h e) -> p c h e", e=96)
        wqs4 = WqS.t.rearrange("p c (h e) -> p c h e", e=96)
        for c in range(3):
            k.cp("pool", wqs4[:, c, :, 64:80], wqp4[:, c, :, 80:96], WqS, [WqP])
            k.cp("pool", wqs4[:, c, :, 80:96], wqp4[:, c, :, 64:80], WqS, [WqP])
        k.dma("sp", wst[:, 0:2, :], w_kvup_d.rearrange("(c p) n -> p c n", p=128), writes=[wst])
        k.memset("pool", WknE[:, :, :], 0.0, WknE)
        wkn4 = WknE.t.rearrange("p c (h e) -> p c h e", e=96)
        wv4 = Wv.t.rearrange("p c (h e) -> p c h e", e=64)
        for c in range(2):
            w4 = wst.t[:, c, :].rearrange("p (h e) -> p h e", e=128)
            k.ts(wkn4[:, c, :, 0:64], w4[:, :, 0:64], gkvT[:, c:c + 1], None, ALU.mult, None, WknE, [wst, gkvT])
            k.ts(wv4[:, c, :, :], w4[:, :, 64:128], gkvT[:, c:c + 1], None, ALU.mult, None, Wv, [wst, gkvT])

        k.barrier()
        wsub.close()
        xg = [k.sb(ph, [128, 4, D], F32, "xg") for _ in range(2)]
        uT = [k.sb(ph, [128, 8, 512], BF16, "uT") for _ in range(2)]
        qk_out = [k.sb(ph, [128, 8, 512], BF16, "qk_out") for _ in range(2)]
        vs_out = [k.sb(ph, [128, 4, 512], BF16, "vs_out") for _ in range(2)]
        latT = k.sb(ph, [128, 5, 512], BF16, "latT")
        sq = k.sb(ph, [128, 5, 512], BF16, "sq")
        cosg = [k.sb(ph, [96, 512], F32, "cosg") for _ in range(2)]
        sing = [k.sb(ph, [96, 512], F32, "sing") for _ in range(2)]
        krope = k.sb(ph, [96, 512], F32, "krope")
        sqrq = k.sb(ph, [128, 512], F32, "sqrq")
        sqrkv = k.sb(ph, [128, 512], F32, "sqrkv")
        rq = k.sb(ph, [128, 512], F32, "rq")
        rkv = k.sb(ph, [128, 512], F32, "rkv")
        cosR = k.sb(ph, [96, 512], F32, "cosR")
        sinR = k.sb(ph, [96, 512], F32, "sinR")
        tA = [k.sb(ph, [96, 512], F32, "tA") for _ in range(2)]
        tB = [k.sb(ph, [96, 512], F32, "tB") for _ in range(2)]
        tC = [k.sb(ph, [96, 512], F32, "tC") for _ in range(2)]
        qm_out = [k.sb(ph, [96, 8, 512], BF16, "qm_out") for _ in range(1)]
        km_out = [k.sb(ph, [96, 8, 512], BF16, "km_out") for _ in range(1)]
        vm_out = [k.sb(ph, [128, 4, 640], BF16, "vm_out") for _ in range(2)]
        cols = [k.sb(ph, [128, 2], F32, "cols") for _ in range(2)]
        banks = [k.ps(ph, [128, 512], F32, "bk") for _ in range(8)]
        nb_ = _rr(banks)
        for v in vm_out:
            k.memset("pool", v[:, :, :], 1.0, v)
        xv = x_d.rearrange("(t p) d -> p t d", p=128)
        k.dma("sp", xg[0][:, :, :], xv[:, 0:4, :], writes=[xg[0]])
        for g in range(NG):
            X = xg[g % 2]
            U = uT[g % 2]
            if g + 1 < NG:
                k.dma("sp", xg[(g + 1) % 2][:, :, :], xv[:, 4 * (g + 1):4 * (g + 2), :], writes=[xg[(g + 1) % 2]])
            cg, sg = cosg[g % 2], sing[g % 2]
            k.dma("sp", cg[:, :], cos_d[:, g * 512:(g + 1) * 512], writes=[cg])
            k.dma("sp", sg[:, :], sin_d[:, g * 512:(g + 1) * 512], writes=[sg])
            for c in range(8):
                b = nb_()
                for t in range(4):
                    k.tr(b[:, t * 128:(t + 1) * 128], X[:, t, c * 128:(c + 1) * 128], identF[:, :], b, [X, identF],
                         signal=(t == 3))
                k.act(U[:, c, :], b[:, :], AF.Identity, U, [b, sc1p, modT], scale=sc1p[:, c:c + 1], bias=modT[:, c:c + 1])
            QK = qk_out[g % 2]
            for blk in range(8):
                b = nb_()
                k.mm(b[:, :], [(Win[:, c, blk * 128:(blk + 1) * 128], U[:, c, :]) for c in range(8)], b, [Win, U])
                k.cp("act" if blk % 2 == 0 else "dve", QK[:, blk, :], b[:, :], QK, [b])
            k.dma("sp", qsb_d[:, :, g * 512:(g + 1) * 512].rearrange("a p t -> p a t"), QK[:, 0:4, :], reads=[QK])
            k.dma("sp", ksb_d[:, :, g * 512:(g + 1) * 512].rearrange("a p t -> p a t"), QK[:, 4:8, :], reads=[QK])
            VS = vs_out[g % 2]
            for t in range(4):
                b = nb_()
                k.mm(b[:, :], [(U[:, c, t * 128:(t + 1) * 128], Win[:, c, C_VSB:C_VSB + 512]) for c in range(8)], b, [Win, U])
                k.cp("dve", VS[:, t, :], b[:, :], VS, [b])
            k.dma("sp", vsb_d[g * 512:(g + 1) * 512, :].rearrange("(t p) n -> p t n", p=128), VS[:, :, :], reads=[VS])
            for i in range(5):
                b = nb_()
                c0 = C_QD + i * 128
                k.mm(b[:, :], [(Win[:, c, c0:c0 + 128], U[:, c, :]) for c in range(8)], b, [Win, U])
                k.cp("act", latT[:, i, :], b[:, :], latT, [b])
                k.act(sq[:, i, :], b[:, :], AF.Square, sq, [b])
            b1 = nb_()
            k.mm(b1[0:96, :], [(WkrE[:, c, :], U[:, c, :]) for c in range(8)], b1, [WkrE, U])
            b2 = nb_()
            k.mm(b2[0:96, :], [(WkrS[:, c, :], U[:, c, :]) for c in range(8)], b2, [WkrS, U])
            k.tt("dve", tA[0][:, :], b1[0:96, :], cg[:, :], ALU.mult, tA[0], [b1, cg])
            k.tt("dve", tB[0][:, :], b2[0:96, :], sg[:, :], ALU.mult, tB[0], [b2, sg])
            k.tt("pool", krope[:, :], tA[0][:, :], tB[0][:, :], ALU.add, krope, [tA[0], tB[0]])
            bq = nb_()
            k.mm(bq[:, :], [(onesB[:, :], sq[:, i, :]) for i in range(3)], bq, [onesB, sq])
            k.act(sqrq[:, :], bq[:, :], AF.Sqrt, sqrq, [bq], scale=1.0 / 384.0, bias=1e-6)
            k.op("dve", lambda v: v.reciprocal(rq[:, :], sqrq[:, :]), reads=[sqrq], writes=[rq])
            bkv = nb_()
            k.mm(bkv[:, :], [(onesB[:, :], sq[:, 3 + i, :]) for i in range(2)], bkv, [onesB, sq])
            k.act(sqrkv[:, :], bkv[:, :], AF.Sqrt, sqrkv, [bkv], scale=1.0 / 256.0, bias=1e-6)
            k.op("dve", lambda v: v.reciprocal(rkv[:, :], sqrkv[:, :]), reads=[sqrkv], writes=[rkv])
            k.tt("pool", cosR[:, :], cg[:, :], rq[0:96, :], ALU.mult, cosR, [cg, rq])
            k.tt("pool", sinR[:, :], sg[:, :], rq[0:96, :], ALU.mult, sinR, [sg, rq])
            VM = vm_out[g % 2]
            vm4 = VM.t.rearrange("p t (h e) -> p t h e", e=80)
            for t in range(4):
                co = cols[t % 2]
                b = nb_()
                k.mm(b[:, 0:1], [(sq[:, 3 + i, t * 128:(t + 1) * 128], onesB[:, 0:1]) for i in range(2)], b, [sq, onesB])
                k.act(co[:, 0:1], b[:, 0:1], AF.Sqrt, co, [b], scale=1.0 / 256.0, bias=1e-6)
                k.op("dve", lambda v, co=co: v.reciprocal(co[:, 1:2], co[:, 0:1]), reads=[co], writes=[co])
                b = nb_()
                k.mm(b[:, :], [(latT[:, 3 + i, t * 128:(t + 1) * 128], Wv[:, i, :]) for i in range(2)], b, [latT, Wv])
                k.ts(vm4[:, t, :, 0:64], b.t[:, :].rearrange("p (h e) -> p h e", e=64), co[:, 1:2], None, ALU.mult, None,
                     VM, [b, co])
            k.dma("sp", vm_d[g * 512:(g + 1) * 512, :].rearrange("(t p) n -> p t n", p=128), VM[:, :, :], reads=[VM])
            QM = qm_out[0]
            KM = km_out[0]
            for h in range(8):
                a, bb_, cc = tA[h % 2], tB[h % 2], tC[h % 2]
                b1 = nb_()
                k.mm(b1[0:96, :], [(WqP[:, i, h * 96:(h + 1) * 96], latT[:, i, :]) for i in range(3)], b1, [WqP, latT])
                b2 = nb_()
                k.mm(b2[0:96, :], [(WqS[:, i, h * 96:(h + 1) * 96], latT[:, i, :]) for i in range(3)], b2, [WqS, latT])
                k.tt("dve", a[:, :], b1[0:96, :], cosR[:, :], ALU.mult, a, [b1, cosR])
                k.tt("dve", bb_[:, :], b2[0:96, :], sinR[:, :], ALU.mult, bb_, [b2, sinR])
                k.tt("pool", QM[:, h, :], a[:, :], bb_[:, :], ALU.add, QM, [a, bb_])
                b3 = nb_()
                k.mm(b3[0:96, :], [(WknE[:, i, h * 96:(h + 1) * 96], latT[:, 3 + i, :]) for i in range(2)], b3, [WknE, latT])
                k.tt("dve", cc[:, :], b3[0:96, :], rkv[0:96, :], ALU.mult, cc, [b3, rkv])
                k.tt("pool", KM[:, h, :], cc[:, :], krope[:, :], ALU.add, KM, [cc, krope])
            k.dma("sp", qm_d[:, :, g * 512:(g + 1) * 512].rearrange("h p t -> p h t"), QM[:, :, :], reads=[QM])
            k.dma("sp", km_d[:, :, g * 512:(g + 1) * 512].rearrange("h p t -> p h t"), KM[:, :, :], reads=[KM])
        k.barrier()
    if debug == "A":
        return k, st

    with contextlib.ExitStack() as ph:
        k.new_phase()
        kT = [k.sb(ph, [128, S], BF16, "kTsb") for _ in range(4)]
        Vs = [k.sb(ph, [128, 8, 512], BF16, "Vsb") for _ in range(4)]
        for p in range(4):
            k.dma("sp", kT[p][:, :], ksb_d[p, :, :], writes=[kT[p]])
        vv = vsb_d.rearrange("(t p) n -> p t n", p=128)
        for i in range(4):
            k.dma("sp", Vs[i][:, :, :], vv[:, 8 * i:8 * i + 8, :], writes=[Vs[i]])
        onesF = k.sb(ph, [128, 1024], F32, "onesF")
        maskS = k.sb(ph, [128, 128], BF16, "maskS")
        mtmp = k.sb(ph, [128, 128], F32, "mtmp")
        k.memset("dve", onesF[:, :], 1e-30, onesF)
        k.memset("pool", mtmp[:, :], 0.0, mtmp)
        k.op("pool", lambda g_: g_.affine_select(out=mtmp[:, :], in_=mtmp[:, :], pattern=[[1, 128]], compare_op=ALU.is_gt,
                                                 fill=NEG_BIG, base=0, channel_multiplier=-1), reads=(), writes=[mtmp])
        k.cp("dve", maskS[:, :], mtmp[:, :], maskS, [mtmp])
        qg = [k.sb(ph, [128, 4, 512], BF16, "qg") for _ in range(2)]
        om = [k.sb(ph, [128, 1024], F32, "om") for _ in range(3)]
        cpb = [k.sb(ph, [128, 1032], F32, "cpb") for _ in range(3)]
        wb = [k.sb(ph, [128, 1024], BF16, "wb") for _ in range(3)]
        wT = [k.sb(ph, [128, 1024], BF16, "wT") for _ in range(3)]
        oout = [k.sb(ph, [128, 4, 512], BF16, "oout") for _ in range(2)]
        zb = [k.ps(ph, [128, 1024], F32, "zb") for _ in range(2)]
        wTp = [k.ps(ph, [128, 1024], BF16, "wTp") for _ in range(2)]
        oslots = [k.ps(ph, [128, 64], F32, "oslot") for i in range(2)]
        chunks = []
        for g in range(NG):
            for h in range(8):
                for j in range(4):
                    qb = 4 * g + j
                    nkb = 32 - qb
                    nch = (nkb + 7) // 8
                    for ci in range(nch):
                        kb0 = qb + 8 * ci
                        nb = min(8, 32 - kb0)
                        chunks.append((g, h, j, ci, nch, kb0, nb))
        if LIMIT is not None:
            chunks = chunks[START:LIMIT]
        N = len(chunks)
        slot_of = {}
        sl = [0]

        def S1(n):
            g, h, j, ci, nch, kb0, nb = chunks[n]
            p, a = h // 2, h % 2
            W = nb * 128
            Q = qg[g % 2]
            if (h == 0 and j == 0 and ci == 0) or n == 0:
                k.dma("sp", Q[:, :, :], qsb_d[:, :, g * 512:(g + 1) * 512].rearrange("a p t -> p a t"), writes=[Q])
            z = zb[n % 2]
            lq = Q[a * 64:(a + 1) * 64, p, j * 128:(j + 1) * 128]
            kt = kT[p]
            if ci == 0:
                k.op("pe", lambda pe: pe.matmul(z[:, 0:128], lhsT=lq, rhs=kt[a * 64:(a + 1) * 64, kb0 * 128:kb0 * 128 + 128],
                                                 start=True, stop=False), reads=[Q, kt], writes=[z], signal=False)
                k.op("pe", lambda pe: pe.matmul(z[:, 0:128], lhsT=identB[:, :], rhs=maskS[:, :], start=False, stop=True),
                     reads=[identB, maskS], writes=[], signal=(W == 128))
                segs = [(c0, min(c0 + 512 - (c0 % 512), W)) for c0 in ([128] if W > 128 else []) + ([512] if W > 512 else [])]
                for si_, (c0, c1) in enumerate(segs):
                    k.op("pe", lambda pe, c0=c0, c1=c1: pe.matmul(z[:, c0:c1], lhsT=lq,
                                                                 rhs=kt[a * 64:(a + 1) * 64, kb0 * 128 + c0:kb0 * 128 + c1],
                                                                 start=True, stop=True), reads=[], writes=[], signal=(si_ == len(segs) - 1))
            else:
                segs = [(0, min(512, W))] + ([(512, W)] if W > 512 else [])
                for si_, (c0, c1) in enumerate(segs):
                    k.op("pe", lambda pe, c0=c0, c1=c1: pe.matmul(z[:, c0:c1], lhsT=lq,
                                                                 rhs=kt[a * 64:(a + 1) * 64, kb0 * 128 + c0:kb0 * 128 + c1],
                                                                 start=True, stop=True),
                         reads=[Q, kt] if si_ == 0 else [], writes=[z] if si_ == 0 else [], signal=(si_ == len(segs) - 1))
            o_ = om[n % 3]
            k.act(o_[:, 0:W], z[:, 0:W], AF.Sigmoid, o_, [z], scale=-0.125)
            c_ = cpb[n % 3]
            if ci == 0:
                k.memset("dve", c_[:, 0:1], 1.0, c_)
            else:
                cprev = cpb[(n - 1) % 3]
                k.cp("dve", c_[:, 0:1], cprev[:, 1024:1025], c_, [cprev])
            k.op("dve", lambda v: v.tensor_tensor_scan(out=c_[:, 1:1 + W], data0=o_[:, 0:W], data1=onesF[:, 0:W],
                                                       initial=c_[:, 0:1], op0=ALU.mult, op1=ALU.max),
                 reads=[o_, onesF], writes=[c_])
            w_ = wb[n % 3]
            k.tt("pool", w_[:, 0:W], c_[:, 0:W], c_[:, 1:1 + W], ALU.subtract, w_, [c_])

        def S2(n):
            g, h, j, ci, nch, kb0, nb = chunks[n]
            W = nb * 128
            w_ = wb[n % 3]
            tp = wTp[n % 2]
            for i in range(nb):
                k.tr(tp[:, i * 128:(i + 1) * 128], w_[:, i * 128:(i + 1) * 128], identB[:, :], tp, [w_, identB],
                     signal=(i == nb - 1))
            k.cp("act" if n % 2 == 0 else "dve", wT[n % 3][:, 0:W], tp[:, 0:W], wT[n % 3], [tp])

        def S3(n):
            g, h, j, ci, nch, kb0, nb = chunks[n]
            if ci == 0:
                slot_of[(g, h, j)] = sl[0] % 2
                sl[0] += 1
            si = slot_of[(g, h, j)]
            osl = oslots[si]
            oap = osl[:, :]
            t_ = wT[n % 3]
            for i in range(nb):
                kb = kb0 + i
                vb = Vs[kb // 8]
                first = (ci == 0 and i == 0)
                last = (ci == nch - 1 and i == nb - 1)
                k.op("pe", lambda pe, i=i, kb=kb, vb=vb, first=first, last=last:
                     pe.matmul(oap, lhsT=t_[:, i * 128:(i + 1) * 128], rhs=vb[:, kb % 8, h * 64:(h + 1) * 64],
                               start=first, stop=last),
                     reads=[t_, vb] if i == 0 else [vb], writes=[osl] if i == 0 else [], signal=(i == nb - 1))
            if ci == nch - 1:
                O = oout[g % 2]
                k.cp("dve", O[:, j, h * 64:(h + 1) * 64], oap, O, [osl])
                if h == 7 and j == 3:
                    for tt_ in range(4):
                        r0 = g * 512 + tt_ * 128
                        k.dma("sp", o_d[r0:r0 + 128, 0:512], O[:, tt_, :], reads=[O])

        for n in range(N + SK3):
            if n < N:
                S1(n)
            if 0 <= n - SK2 < N:
                S2(n - SK2)
            if 0 <= n - SK3 < N:
                S3(n - SK3)
        for _ in range(PAD):
            k.memset("dve", mtmp[:, 0:8], 0.0, mtmp)
        k.barrier()
    if debug == "B1s":
        return k, st

    with contextlib.ExitStack() as ph:
        k.new_phase()
        kM = [k.sb(ph, [96, S], BF16, "kM") for _ in range(8)]
        Vm = [k.sb(ph, [128, 8, 640], BF16, "Vm") for _ in range(4)]
        for h in range(8):
            k.dma("sp", kM[h][:, :], km_d[h, :, :], writes=[kM[h]])
        vv = vm_d.rearrange("(t p) n -> p t n", p=128)
        for i in range(4):
            k.dma("sp", Vm[i][:, :, :], vv[:, 8 * i:8 * i + 8, :], writes=[Vm[i]])
        maskM = k.sb(ph, [128, 128], BF16, "maskM")
        zB = k.sb(ph, [128, 128], BF16, "zB")
        k.memset("dve", zB[:, :], 0.0, zB)
        mtmp = k.sb(ph, [128, 128], F32, "mtmp2")
        k.memset("pool", mtmp[:, :], 1.0, mtmp)
        k.op("pool", lambda g_: g_.affine_select(out=mtmp[:, :], in_=mtmp[:, :], pattern=[[-1, 128]], compare_op=ALU.is_gt,
                                                 fill=0.0, base=1, channel_multiplier=1), reads=(), writes=[mtmp])
        k.cp("dve", maskM[:, :], mtmp[:, :], maskM, [mtmp])
        qm = [k.sb(ph, [96, 8, 512], BF16, "qm") for _ in range(2)]
        pT = [k.sb(ph, [128, 512], BF16, "pT") for _ in range(5)]
        oout = [k.sb(ph, [128, 4, 512], BF16, "ooutm") for _ in range(2)]
        rc = [k.sb(ph, [128, 8], F32, "rc") for _ in range(2)]
        sb_ = [k.ps(ph, [128, 512], F32, "sTb") for _ in range(3)]
        oM = [k.ps(ph, [128, 512], F32, "oM") for _ in range(2)]
        SC = 1.0 / (96.0 ** 0.5)
        blocks = []
        for g in range(NG):
            for h in range(8):
                for kb in range(4 * g, 32):
                    blocks.append((g, h, kb))
        if MLIMIT is not None:
            blocks = blocks[:MLIMIT]
        N = len(blocks)

        def M1(n):
            g, h, kb = blocks[n]
            m = kb - 4 * g
            Q = qm[g % 2]
            if h == 0 and m == 0:
                k.dma("sp", Q[:, :, :], qm_d[:, :, g * 512:(g + 1) * 512].rearrange("h p t -> p h t"), writes=[Q])
            s_ = sb_[n % 3]
            kk = kM[h]
            lk = kk[:, kb * 128:(kb + 1) * 128]
            if m >= 4:
                ncol = 512
                k.op("pe", lambda pe: pe.matmul(s_[:, 0:512], lhsT=lk, rhs=Q[:, h, :], start=True, stop=True),
                     reads=[kk, Q], writes=[s_], signal=True)
            else:
                ncol = (m + 1) * 128
                if m > 0:
                    k.op("pe", lambda pe: pe.matmul(s_[:, 0:m * 128], lhsT=lk, rhs=Q[:, h, 0:m * 128], start=True, stop=True),
                         reads=[kk, Q], writes=[s_], signal=False)
                k.op("pe", lambda pe: pe.matmul(s_[:, m * 128:ncol], lhsT=lk, rhs=Q[:, h, m * 128:ncol], start=True, stop=True),
                     reads=[kk, Q], writes=[s_], signal=True)
            k.act(pT[n % 5][:, 0:ncol], s_[:, 0:ncol], AF.Exp, pT[n % 5], [s_], scale=SC)
            if m < 4:
                p__ = pT[n % 5]
                k.tt("dve", p__[:, m * 128:ncol], p__[:, m * 128:ncol], maskM[:, :], ALU.mult, p__, [p__, maskM])

        def M2(n):
            g, h, kb = blocks[n]
            m = kb - 4 * g
            acc = oM[(g * 8 + h) % 2]
            acc4 = acc.t[:, 0:320].rearrange("p (j e) -> p j e", e=80)
            p_ = pT[n % 5]
            vb = Vm[kb // 8]
            vap = vb.t[:, kb % 8, :].rearrange("p (hh e) -> p hh e", e=80)
            nj = min(m, 3) + 1
            if m == 0:
                k.op("pe", lambda pe: pe.matmul(acc.t[:, 0:320], lhsT=zB[:, :], rhs=Vm[0].t[:, 0, 0:320], start=True, stop=False),
                     reads=[zB, Vm[0]], writes=[acc], signal=False)
            for j in range(nj):
                first = False
                last = (kb == 31)
                k.op("pe", lambda pe, j=j, first=first, last=last:
                     pe.matmul(acc4[:, j, 0:66], lhsT=p_[:, j * 128:(j + 1) * 128], rhs=vap[:, h, 0:66], start=first, stop=last),
                     reads=[p_, vb] if j == 0 else [], writes=[acc] if j == 0 else [], signal=(j == nj - 1))
            if kb == 31 and not NOFIN:
                O = oout[g % 2]
                r_ = rc[(g * 8 + h) % 2]
                for j in range(4):
                    k.cp("dve", r_[:, 4 + j:5 + j], acc4[:, j, 64:65], r_, [acc])
                if not DBGM:
                    k.op("dve", lambda v: v.reciprocal(r_[:, 0:4], r_[:, 4:8]), reads=[r_], writes=[r_])
                for j in range(4):
                    if DBGM:
                        k.cp("dve", O[:, j, h * 64:(h + 1) * 64], acc4[:, j, 1:65], O, [acc])
                    else:
                        k.ts(O[:, j, h * 64:(h + 1) * 64], acc4[:, j, 0:64], r_[:, j:j + 1], None, ALU.mult, None, O, [acc, r_])
                if DBGM and MLIMIT is not None and h == 1:
                    for tt_ in range(4):
                        r0 = g * 512 + tt_ * 128
                        k.dma("sp", o_d[r0:r0 + 128, 512:1024], O[:, tt_, :], reads=[O])
                if h == 7:
                    for tt_ in range(4):
                        r0 = g * 512 + tt_ * 128
                        k.dma("sp", o_d[r0:r0 + 128, 512:1024], O[:, tt_, :], reads=[O])

        for n in range(N + 3):
            if n < N:
                M1(n)
            if 0 <= n - 3 < N:
                M2(n - 3)
        k.barrier()
    if debug == "B1":
        return k, st

    with contextlib.ExitStack() as ph:
        k.new_phase()
        wvin = w_in_d.rearrange("(c p) n -> p c n", p=128)
        Wgi = k.sb(ph, [128, 8, 2048], BF16, "Wgi")
        for c in range(8):
            k.dma("pool", Wgi[:, c, :], wvin[:, c, C_GSB:C_GSB + 2048], writes=[Wgi])
        Wb = k.sb(ph, [128, 8, D], BF16, "Wb")
        k.dma("pool", Wb[:, 0:4, :], w_bsb_d.rearrange("(c p) n -> p c n", p=128), writes=[Wb])
        k.dma("pool", Wb[:, 4:8, :], w_bmla_d.rearrange("(c p) n -> p c n", p=128), writes=[Wb])
        Wo = k.sb(ph, [128, 8, D], BF16, "Wo")
        k.dma("pool", Wo[:, :, :], w_out_d.rearrange("(c p) n -> p c n", p=128), writes=[Wo])
        Wr = k.sb(ph, [128, 8, 36], F32, "Wr")
        k.dma("sp", Wr[:, :, 0:4], w_rg_d.rearrange("(c p) n -> p c n", p=128), writes=[Wr])
        k.dma("sp", Wr[:, :, 4:36], w_re_d.rearrange("(c p) n -> p c n", p=128), writes=[Wr])
        brow = k.sb(ph, [128, 36], F32, "brow")
        k.dma("sp", brow[:, 0:4], b_rg_d[0:1, :].partition_broadcast(128), writes=[brow])
        k.dma("sp", brow[:, 4:36], b_re_d[0:1, :].partition_broadcast(128), writes=[brow])
        g1bc = k.sb(ph, [128, D], F32, "g1bc")
        k.dma("sp", g1bc[:, :], mod_d[0:1, 2 * D:3 * D].partition_broadcast(128), writes=[g1bc])
        gA = k.sb(ph, [128, D], F32, "gA")
        bA = k.sb(ph, [128, D], F32, "bA")
        k.dma("sp", gA[:, :], ln1g_d[0:1, :].partition_broadcast(128), writes=[gA])
        k.dma("sp", bA[:, :], ln1b_d[0:1, :].partition_broadcast(128), writes=[bA])
        k.ts(gA[:, :], gA[:, :], ALPHA, None, ALU.mult, None, gA, [gA])
        k.ts(bA[:, :], bA[:, :], ALPHA, None, ALU.mult, None, bA, [bA])
        lgT = k.sb(ph, [128, 8], F32, "lgT")
        lbT = k.sb(ph, [128, 8], F32, "lbT")
        A2 = k.sb(ph, [128, 8], F32, "A2")
        B2 = k.sb(ph, [128, 8], F32, "B2")
        k.dma("sp", lgT[:, :], ln1g_d.rearrange("o (c p) -> p (o c)", p=128), writes=[lgT])
        k.dma("sp", lbT[:, :], ln1b_d.rearrange("o (c p) -> p (o c)", p=128), writes=[lbT])
        k.ts(A2[:, :], modT[:, 32:40], 1.0, None, ALU.add, None, A2, [modT])
        k.tt("dve", B2[:, :], lbT[:, :], A2[:, :], ALU.mult, B2, [lbT, A2])
        k.tt("dve", B2[:, :], B2[:, :], modT[:, 24:32], ALU.add, B2, [B2, modT])
        k.tt("dve", A2[:, :], A2[:, :], lgT[:, :], ALU.mult, A2, [A2, lgT])

        xg = k.sb(ph, [128, 4, D], F32, "xg2")
        og = k.sb(ph, [128, 4, D], BF16, "og")
        uT = k.sb(ph, [128, 8, 512], BF16, "uT2")
        gates = k.sb(ph, [128, 4, 2048], BF16, "gates")
        oT = [k.sb(ph, [128, D], BF16, "oT") for _ in range(2)]
        t1 = k.sb(ph, [128, D], F32, "t1")
        t2 = k.sb(ph, [128, D], F32, "t2")
        mixed = k.sb(ph, [128, D], BF16, "mixed")
        mT = [k.sb(ph, [128, D], BF16, "mT") for _ in range(2)]
        pre = k.sb(ph, [128, 4, D], F32, "pre")
        acc0 = [k.sb(ph, [128, D], F32, "acc0") for _ in range(2)]
        u2T = k.sb(ph, [128, 8, 512], F32, "u2T")
        u2Tb = k.sb(ph, [128, 8, 512], BF16, "u2Tb")
        stt = k.sb(ph, [128, 4, 12], F32, "stt")
        mv = k.sb(ph, [128, 4, 2], F32, "mv")
        sd = k.sb(ph, [128, 4, 2], F32, "sd")
        comb = k.sb(ph, [128, 4, 32], F32, "comb")
        rs = [k.sb(ph, [128, 160], F32, "rs") for _ in range(2)]
        fb = [k.ps(ph, [128, 512], F32, "fb") for _ in range(2)]
        tpb = [k.ps(ph, [128, D], BF16, "tpb") for _ in range(2)]
        yp = [k.ps(ph, [128, D], F32, "yp") for _ in range(2)]
        nfb = _rr(fb)
        ntp = _rr(tpb)
        xv = x_d.rearrange("(t p) d -> p t d", p=128)
        ov = o_d.rearrange("(t p) d -> p t d", p=128)
        for G in range(NG):
            k.dma("sp", xg[:, :, :], xv[:, 4 * G:4 * G + 4, :], writes=[xg])
            k.dma("sp", og[:, :, :], ov[:, 4 * G:4 * G + 4, :], writes=[og])
            for c in range(8):
                b = nfb()
                for t in range(4):
                    k.tr(b[:, t * 128:(t + 1) * 128], xg[:, t, c * 128:(c + 1) * 128], identF[:, :], b, [xg, identF], signal=(t == 3))
                k.act(uT[:, c, :], b[:, :], AF.Identity, uT, [b, sc1p, modT], scale=sc1p[:, c:c + 1], bias=modT[:, c:c + 1])
            for t in range(4):
                for blk in range(4):
                    b = nfb()
                    k.mm(b[:, :], [(uT[:, c, t * 128:(t + 1) * 128], Wgi[:, c, blk * 512:(blk + 1) * 512]) for c in range(8)], b, [uT, Wgi])
                    k.act(gates[:, t, blk * 512:(blk + 1) * 512], b[:, :], AF.Sigmoid, gates, [b])
            for t in range(4):
                tp = ntp()
                for c in range(8):
                    k.tr(tp[:, c * 128:(c + 1) * 128], og[:, t, c * 128:(c + 1) * 128], identB[:, :], tp, [og, identB], signal=(c == 7))
                o_ = oT[t % 2]
                k.cp("dve", o_[:, :], tp[:, :], o_, [tp])
                ysb, yml = yp[0], yp[1]
                for hf in range(2):
                    k.mm(ysb[:, hf * 512:(hf + 1) * 512], [(o_[:, c * 128:(c + 1) * 128], Wb[:, c, hf * 512:(hf + 1) * 512]) for c in range(4)],
                         ysb, [o_, Wb], signal=(hf == 1))
                for hf in range(2):
                    k.mm(yml[:, hf * 512:(hf + 1) * 512], [(o_[:, c * 128:(c + 1) * 128], Wb[:, c, hf * 512:(hf + 1) * 512]) for c in range(4, 8)],
                         yml, [o_, Wb], signal=(hf == 1))
                k.tt("dve", t1[:, :], ysb[:, :], gates[:, t, 0:D], ALU.mult, t1, [ysb, gates])
                k.tt("dve", t2[:, :], yml[:, :], gates[:, t, D:2 * D], ALU.mult, t2, [yml, gates])
                k.tt("pool", mixed[:, :], t1[:, :], t2[:, :], ALU.add, mixed, [t1, t2])
                tp = ntp()
                for c in range(8):
                    k.tr(tp[:, c * 128:(c + 1) * 128], mixed[:, c * 128:(c + 1) * 128], identB[:, :], tp, [mixed, identB], signal=(c == 7))
                m_ = mT[t % 2]
                k.cp("act", m_[:, :], tp[:, :], m_, [tp])
                at = ysb
                for hf in range(2):
                    k.mm(at[:, hf * 512:(hf + 1) * 512], [(m_[:, c * 128:(c + 1) * 128], Wo[:, c, hf * 512:(hf + 1) * 512]) for c in range(8)],
                         at, [m_, Wo], signal=(hf == 1))
                k.tt("dve", t1[:, :], at[:, :], g1bc[:, :], ALU.mult, t1, [at, g1bc])
                k.op("dve", lambda v, t=t: v.scalar_tensor_tensor(out=pre[:, t, :], in0=xg[:, t, :], scalar=ALPHA, in1=t1[:, :],
                                                                    op0=ALU.mult, op1=ALU.add), reads=[xg, t1], writes=[pre])
                for hf in range(2):
                    k.op("dve", lambda v, t=t, hf=hf: v.bn_stats(out=stt[:, t, hf * 6:(hf + 1) * 6], in_=pre[:, t, hf * 512:(hf + 1) * 512]),
                         reads=[pre], writes=[stt])
                k.op("dve", lambda v, t=t: v.bn_aggr(out=mv[:, t, :], in_=stt[:, t, :].rearrange("p (a b) -> p a b", b=6)),
                     reads=[stt], writes=[mv])
            for t in range(4):
                k.act(sd[:, t, 0:1], mv[:, t, 1:2], AF.Sqrt, sd, [mv], scale=1.0, bias=1e-5)
                k.op("dve", lambda v, t=t: v.reciprocal(sd[:, t, 1:2], sd[:, t, 0:1]), reads=[sd], writes=[sd])
                k.ts(pre[:, t, :], pre[:, t, :], mv[:, t, 0:1], sd[:, t, 1:2], ALU.subtract, ALU.mult, pre, [pre, mv, sd])
                a_ = acc0[t % 2]
                k.tt("pool", a_[:, :], pre[:, t, :], gA[:, :], ALU.mult, a_, [pre, gA])
                k.tt("pool", a_[:, :], a_[:, :], bA[:, :], ALU.add, a_, [a_, bA])
                k.dma("sp", acc_d[(4 * G + t) * 128:(4 * G + t + 1) * 128, :], a_[:, :], reads=[a_])
            for c in range(8):
                b = nfb()
                for t in range(4):
                    k.tr(b[:, t * 128:(t + 1) * 128], pre[:, t, c * 128:(c + 1) * 128], identF[:, :], b, [pre, identF], signal=(t == 3))
                k.act(u2T[:, c, :], b[:, :], AF.Identity, u2T, [b, A2, B2], scale=A2[:, c:c + 1], bias=B2[:, c:c + 1])
            k.cp("pool", u2Tb[:, :, :], u2T[:, :, :], u2Tb, [u2T])
            k.dma("sp", u2T_d[:, :, G * 512:(G + 1) * 512].rearrange("c p t -> p c t"), u2Tb[:, :, :], reads=[u2Tb])
            for t in range(4):
                b = nfb()
                r_ = rs[t % 2]
                k.mm(b[:, 0:36], [(u2T[:, c, t * 128:(t + 1) * 128], Wr[:, c, :]) for c in range(8)], b, [u2T, Wr])
                lg = r_[:, 0:36]
                k.tt("dve", lg, b[:, 0:36], brow[:, :], ALU.add, r_, [b, brow])
                gmax, ngm, sg4, om4, e4, ssum, pg = (r_[:, 40:41], r_[:, 41:42], r_[:, 44:48], r_[:, 48:52], r_[:, 52:56],
                                                     r_[:, 42:43], r_[:, 43:44])
                oh, elm, top8 = r_[:, 56:60], r_[:, 64:96], r_[:, 96:104]
                dd, sgd, w1, w2, c1 = r_[:, 104:105], r_[:, 105:106], r_[:, 106:107], r_[:, 107:108], r_[:, 112:144]
                R = [r_]
                k.op("dve", lambda v: v.tensor_reduce(out=gmax, in_=r_[:, 0:4], axis=AX.X, op=ALU.max), reads=R, writes=R)
                k.ts(ngm, gmax, -1.0, None, ALU.mult, None, r_, R)
                k.act(sg4, r_[:, 0:4], AF.Sigmoid, r_, R, bias=ngm)
                k.ts(om4, sg4, -1.0, 1.0, ALU.mult, ALU.add, r_, R)
                k.op("dve", lambda v: v.reciprocal(om4, om4), reads=R, writes=R)
                k.tt("dve", e4, sg4, om4, ALU.mult, r_, R)
                k.op("dve", lambda v: v.tensor_reduce(out=ssum, in_=e4, axis=AX.X, op=ALU.add), reads=R, writes=R)
                k.op("dve", lambda v: v.reciprocal(pg, ssum), reads=R, writes=R)
                k.ts(oh, r_[:, 0:4], gmax, None, ALU.is_equal, None, r_, R)
                k.ts(oh, oh, 1e30, -1e30, ALU.mult, ALU.add, r_, R)
                for gi in range(4):
                    k.ts(r_[:, 64 + 8 * gi:72 + 8 * gi], r_[:, 4 + 8 * gi:12 + 8 * gi], r_[:, 56 + gi:57 + gi], None, ALU.add, None, r_, R)
                k.op("dve", lambda v: v.max(out=top8, in_=elm), reads=R, writes=R)
                k.tt("dve", dd, r_[:, 96:97], r_[:, 97:98], ALU.subtract, r_, R)
                k.act(sgd, dd, AF.Sigmoid, r_, R)
                k.tt("dve", w1, pg, sgd, ALU.mult, r_, R)
                k.tt("dve", w2, pg, w1, ALU.subtract, r_, R)
                k.ts(c1, elm, r_[:, 96:97], w1, ALU.is_equal, ALU.mult, r_, R)
                k.ts(comb[:, t, :], elm, r_[:, 97:98], w2, ALU.is_equal, ALU.mult, comb, R)
                k.tt("dve", comb[:, t, :], comb[:, t, :], c1, ALU.add, comb, [comb, r_])
            k.dma("sp", comb_d[G * 512:(G + 1) * 512, :].rearrange("(t p) n -> p t n", p=128), comb[:, :, :], reads=[comb])
        k.barrier()
    if debug == "B2":
        return k, st

    with contextlib.ExitStack() as ph:
        k.new_phase()
        g2bc = k.sb(ph, [128, D], F32, "g2bc")
        l2g = k.sb(ph, [128, D], F32, "l2g")
        l2b = k.sb(ph, [128, D], F32, "l2b")
        k.dma("sp", g2bc[:, :], mod_d[0:1, 5 * D:6 * D].partition_broadcast(128), writes=[g2bc])
        k.dma("sp", l2g[:, :], ln2g_d[0:1, :].partition_broadcast(128), writes=[l2g])
        k.dma("sp", l2b[:, :], ln2b_d[0:1, :].partition_broadcast(128), writes=[l2b])
        acc = k.sb(ph, [128, 16, D], F32, "acc")
        u2 = k.sb(ph, [128, 8, 2048], BF16, "u2")
        cmb = k.sb(ph, [128, 16, 32], F32, "cmb")
        Wg = [k.sb(ph, [128, 8, 256], BF16, "Wg") for _ in range(2)]
        Wu = [k.sb(ph, [128, 8, 256], BF16, "Wu") for _ in range(2)]
        Wds = [k.sb(ph, [128, 2, D], F32, "Wds") for _ in range(2)]
        Wd = [k.sb(ph, [128, 2, D], BF16, "Wd") for _ in range(2)]
        sgb = [k.sb(ph, [128, 512], F32, "sgb") for _ in range(2)]
        hT = [k.sb(ph, [128, 2, 512], BF16, "hT") for _ in range(2)]
        stt = k.sb(ph, [128, 12], F32, "stt2")
        mv = k.sb(ph, [128, 16, 2], F32, "mv2")
        sd = k.sb(ph, [128, 16, 2], F32, "sd2")
        yo = [k.sb(ph, [128, D], F32, "yo") for _ in range(2)]
        gub = [k.ps(ph, [128, 512], F32, "gub") for _ in range(4)]
        dnb = [k.ps(ph, [128, D], F32, "dnb") for _ in range(2)]
        ngu = _rr(gub)
        ndn = _rr(dnb)
        NE = 32
        for ps_ in range(2):
            T0 = ps_ * 2048
            k.dma("sp", acc[:, :, :], acc_d[T0:T0 + 2048, :].rearrange("(t p) d -> p t d", p=128), writes=[acc])
            k.dma("sp", u2[:, :, :], u2T_d[:, :, T0:T0 + 2048].rearrange("c p t -> p c t"), writes=[u2])
            k.dma("sp", cmb[:, :, :], comb_d[T0:T0 + 2048, :].rearrange("(t p) n -> p t n", p=128), writes=[cmb])
            pend = None
            seq = [(e, gr) for e in range(NE) for gr in range(4)]

            def load_w(e):
                i = e % 2
                k.dma("pool", Wg[i][:, :, :], w_eg_d[e].rearrange("(c p) f -> p c f", p=128), writes=[Wg[i]])
                k.dma("pool", Wu[i][:, :, :], w_eu_d[e].rearrange("(c p) f -> p c f", p=128), writes=[Wu[i]])
                k.dma("sp", Wds[i][:, :, :], w_ed_d[e].rearrange("(c p) d -> p c d", p=128), writes=[Wds[i]])
                for c in range(2):
                    k.tt("pool", Wd[i][:, c, :], Wds[i][:, c, :], g2bc[:, :], ALU.mult, Wd[i], [Wds[i], g2bc])

            def gate_up(e, gr, it):
                i = e % 2
                H = hT[it % 2]
                for fc in range(2):
                    bg = ngu()
                    k.mm(bg[:, :], [(Wg[i][:, c, fc * 128:(fc + 1) * 128], u2[:, c, gr * 512:(gr + 1) * 512]) for c in range(8)], bg, [Wg[i], u2])
                    bu = ngu()
                    k.mm(bu[:, :], [(Wu[i][:, c, fc * 128:(fc + 1) * 128], u2[:, c, gr * 512:(gr + 1) * 512]) for c in range(8)], bu, [Wu[i], u2])
                    sg_ = sgb[fc]
                    k.act(sg_[:, :], bg[:, :], AF.Silu, sg_, [bg])
                    k.tt("dve", H[:, fc, :], sg_[:, :], bu[:, :], ALU.mult, H, [sg_, bu])

            def down(e, gr, it):
                i = e % 2
                H = hT[it % 2]
                for t in range(4):
                    tile = gr * 4 + t
                    bd = ndn()
                    for hf in range(2):
                        k.mm(bd[:, hf * 512:(hf + 1) * 512], [(H[:, fc, t * 128:(t + 1) * 128], Wd[i][:, fc, hf * 512:(hf + 1) * 512]) for fc in range(2)],
                             bd, [H, Wd[i]], signal=(hf == 1))
                    k.op("dve", lambda v, tile=tile, bd=bd: v.scalar_tensor_tensor(out=acc[:, tile, :], in0=bd[:, :], scalar=cmb[:, tile, e:e + 1],
                                                                                  in1=acc[:, tile, :], op0=ALU.mult, op1=ALU.add),
                         reads=[bd, cmb, acc], writes=[acc])

            load_w(0)
            for it, (e, gr) in enumerate(seq):
                gate_up(e, gr, it)
                if pend is not None:
                    down(*pend)
                pend = (e, gr, it)
                if gr == 0 and e + 1 < NE:
                    load_w(e + 1)
            down(*pend)
            for t in range(16):
                for hf in range(2):
                    k.op("dve", lambda v, t=t, hf=hf: v.bn_stats(out=stt[:, hf * 6:(hf + 1) * 6], in_=acc[:, t, hf * 512:(hf + 1) * 512]),
                         reads=[acc], writes=[stt])
                k.op("dve", lambda v, t=t: v.bn_aggr(out=mv[:, t, :], in_=stt[:, :].rearrange("p (a b) -> p a b", b=6)),
                     reads=[stt], writes=[mv])
            for t in range(16):
                k.act(sd[:, t, 0:1], mv[:, t, 1:2], AF.Sqrt, sd, [mv], scale=1.0, bias=1e-5)
                k.op("dve", lambda v, t=t: v.reciprocal(sd[:, t, 1:2], sd[:, t, 0:1]), reads=[sd], writes=[sd])
                y_ = yo[t % 2]
                k.ts(y_[:, :], acc[:, t, :], mv[:, t, 0:1], sd[:, t, 1:2], ALU.subtract, ALU.mult, y_, [acc, mv, sd])
                k.tt("pool", y_[:, :], y_[:, :], l2g[:, :], ALU.mult, y_, [y_, l2g])
                k.tt("pool", y_[:, :], y_[:, :], l2b[:, :], ALU.add, y_, [y_, l2b])
                k.dma("sp", out_d[T0 + t * 128:T0 + (t + 1) * 128, :], y_[:, :], reads=[y_])
            k.barrier()
    return k, st


def _prep_inputs(inputs):
    f = lambda a: np.ascontiguousarray(np.asarray(a))
    x = f(inputs["x"])[:, ::-1, :]
    pos = f(inputs["positions"])[:, ::-1]
    inv = (1.0 / (10000.0 ** (np.arange(0, 32, 2, dtype=np.float32) / 32.0))).astype(np.float32)
    cst = np.zeros((128, 4), np.float32)
    cst[64:80, 0] = inv
    cst[80:96, 0] = inv
    cst[64:80, 1] = -1.0
    cst[80:96, 1] = 1.0
    shared = {"cst": cst}
    for name in ("w_ada", "b_ada", "w_in", "mla_q_norm_g", "w_q_up", "mla_kv_norm_g", "w_kv_up", "w_branch_sb",
                 "w_branch_mla", "w_out", "ln1_g", "ln1_b", "w_router_group", "b_router_group", "w_router_expert",
                 "b_router_expert", "w_exp_gate", "w_exp_up", "w_exp_down", "ln2_g", "ln2_b"):
        a = f(inputs[name])
        a = a.reshape(a.shape[1:]) if a.ndim >= 3 else a
        shared[name] = np.ascontiguousarray(a)
    maps = []
    for b in range(8):
        m = dict(shared)
        m["x"] = np.ascontiguousarray(x[b])
        m["c"] = np.ascontiguousarray(f(inputs["c"])[b:b + 1])
        m["positions"] = np.ascontiguousarray(pos[b:b + 1]).astype(np.int32)
        maps.append(m)
    return maps


def kernel(**inputs):
    maps = _prep_inputs(inputs)
    k, st = build()
    res = run_bass_kernel_spmd(k.nc, maps, core_ids=list(range(8)))
    out = np.stack([np.asarray(r["out"]) for r in res.results], axis=0)
    return np.ascontiguousarray(out[:, ::-1, :]).astype(np.float32)
```

```python
import contextlib
import numpy as np
import concourse.bass as bass
import concourse.mybir as mybir
from concourse.bass_utils import run_bass_kernel_spmd

F32 = mybir.dt.float32
BF16 = mybir.dt.bfloat16
I32 = mybir.dt.int32
ALU = mybir.AluOpType
AF = mybir.ActivationFunctionType
AX = mybir.AxisListType

S = 4096
D = 1024
NT = 32
NG = 8
C_QSB, C_KSB, C_VSB, C_QD, C_KVD, C_KR, C_GSB, C_GMLA = 0, 512, 1024, 1536, 1920, 2176, 2208, 3232
ALPHA = 2.0 ** 0.25
MAGIC = 12582912.0
TWO_PI = 6.283185307179586
NEG_BIG = -30000.0
SAME_ENGINE_SYNC = True
LIMIT = None
PAD = 0
START = 0
MLIMIT = None
DBGM = False
NOFIN = False
SK2, SK3 = 1, 2


class Buf:
    __slots__ = ("t", "w", "r", "ds", "sw", "name")

    def __init__(self, t, name=""):
        self.t = t
        self.w = None
        self.r = {}
        self.ds = None
        self.sw = None
        self.name = name

    def __getitem__(self, k):
        return self.t[k]


class DSem:
    __slots__ = ("sem", "count")

    def __init__(self, sem):
        self.sem = sem
        self.count = 0


class KB:
    def __init__(self):
        nc = bass.Bass("TRN2", target_bir_lowering=False)
        self.nc = nc
        self.eng = {"pe": nc.tensor, "act": nc.scalar, "dve": nc.vector, "pool": nc.gpsimd, "sp": nc.sync}
        self.root = contextlib.ExitStack()
        self.esem = {}
        self.tick = {}
        for e in ("pe", "act", "dve", "pool"):
            self.esem[e] = self.root.enter_context(nc.semaphore("es_" + e))
            self.tick[e] = 0
        self.waited = {e: {} for e in self.eng}
        self.dpool = [DSem(self.root.enter_context(nc.semaphore("ds%d" % i))) for i in range(80)]
        self.swpool = [DSem(self.root.enter_context(nc.semaphore("sw%d" % i))) for i in range(8)]
        self.swnext = 0
        self.dnext = 0
        self.dbase = None
        self.uid = 0

    def sb(self, st, shape, dt, name=None):
        self.uid += 1
        name = "%s_%d" % (name or "t", self.uid)
        return Buf(st.enter_context(self.nc.sbuf_tensor(name, shape, dt)), name)

    def ps(self, st, shape, dt, name=None):
        self.uid += 1
        name = "%s_%d" % (name or "p", self.uid)
        return Buf(st.enter_context(self.nc.psum_tensor(name, shape, dt)), name)

    def view(self, buf, name=""):
        return Buf(buf.t, name)

    def new_phase(self):
        if self.dbase is None:
            self.dbase = self.dnext
        self.dnext = self.dbase

    def _dsem(self, b):
        if b.ds is None:
            b.ds = self.dpool[self.dnext]
            self.dnext += 1
            if self.dnext >= len(self.dpool):
                self.dnext = self.dbase or 0
        return b.ds

    def _sync(self, e, reads, writes):
        need = {}
        wd = self.waited[e]

        def add(m):
            if m is None:
                return
            sem, val, src = m
            if src == e and (e == "pe" or not SAME_ENGINE_SYNC):
                return
            key = id(sem)
            if wd.get(key, 0) >= val:
                return
            if key not in need or need[key][1] < val:
                need[key] = (sem, val)

        for b in reads:
            add(b.w)
        for b in writes:
            add(b.w)
            for m in b.r.values():
                add(m)
        for key, (sem, val) in need.items():
            self.eng[e].wait_ge(sem, val)
            wd[key] = val

    def op(self, e, fn, reads=(), writes=(), signal=True):
        self._sync(e, reads, writes)
        ins = fn(self.eng[e])
        if signal:
            self.tick[e] += 1
            ins.then_inc(self.esem[e], 1)
            m = (self.esem[e], self.tick[e], e)
        else:
            m = (self.esem[e], self.tick[e] + 1, e)
        k = id(m[0])
        for b in reads:
            b.r[k] = m
        for b in writes:
            b.w = m
            b.r = {}
        return ins

    def dma(self, q, out, in_, reads=(), writes=()):
        self._sync(q, reads, writes)
        ins = self.eng[q].dma_start(out=out, in_=in_)
        owner = writes[0] if writes else reads[0]
        if q == "pool":
            if owner.sw is None:
                owner.sw = self.swpool[self.swnext]
                self.swnext += 1
            ds = owner.sw
        else:
            ds = self._dsem(owner)
        ds.count += 16
        ins.then_inc(ds.sem, 16)
        m = (ds.sem, ds.count, "dma")
        k = id(ds.sem)
        for b in reads:
            b.r[k] = m
        for b in writes:
            b.w = m
            b.r = {}
        return ins

    def barrier(self):
        for e in self.eng:
            for o in ("pe", "act", "dve", "pool"):
                if o != e and self.tick[o] > self.waited[e].get(id(self.esem[o]), 0):
                    self.eng[e].wait_ge(self.esem[o], self.tick[o])
                    self.waited[e][id(self.esem[o])] = self.tick[o]
            for ds in self.dpool + self.swpool:
                if ds.count > self.waited[e].get(id(ds.sem), 0):
                    self.eng[e].wait_ge(ds.sem, ds.count)
                    self.waited[e][id(ds.sem)] = ds.count

    def mm(self, out_ap, pairs, outb, inbs, start=True, stop=True, signal=True):
        n = len(pairs)
        for i, (l, r) in enumerate(pairs):
            st = start and i == 0
            sp = stop and i == n - 1
            self.op("pe", lambda pe, l=l, r=r, st=st, sp=sp: pe.matmul(out_ap, lhsT=l, rhs=r, start=st, stop=sp),
                    reads=inbs if i == 0 else (), writes=[outb] if i == 0 else (),
                    signal=(signal and i == n - 1))
        if n and not signal:
            pass

    def tr(self, out_ap, in_ap, ident_ap, outb, inbs, signal=True):
        self.op("pe", lambda pe: pe.transpose(out_ap, in_ap, ident_ap), reads=inbs, writes=[outb], signal=signal)

    def act(self, out, in_, func, outb, inbs, scale=1.0, bias=None, accum_out=None, extra_w=()):
        kw = {}
        if bias is not None:
            kw["bias"] = bias
        if accum_out is not None:
            kw["accum_out"] = accum_out
        return self.op("act", lambda a: a.activation(out=out, in_=in_, func=func, scale=scale, **kw),
                       reads=inbs, writes=[outb] + list(extra_w))

    def tt(self, e, out, in0, in1, op, outb, inbs):
        return self.op(e, lambda v: v.tensor_tensor(out=out, in0=in0, in1=in1, op=op), reads=inbs, writes=[outb])

    def ts(self, out, in0, s1, s2, op0, op1, outb, inbs):
        if op1 is None:
            return self.op("dve", lambda v: v.tensor_single_scalar(out=out, in_=in0, scalar=s1, op=op0),
                           reads=inbs, writes=[outb])
        return self.op("dve", lambda v: v.tensor_scalar(out=out, in0=in0, scalar1=s1, scalar2=s2, op0=op0, op1=op1),
                       reads=inbs, writes=[outb])

    def cp(self, e, out, in_, outb, inbs):
        if e == "act":
            return self.act(out, in_, AF.Copy, outb, inbs)
        return self.op(e, lambda v: v.tensor_copy(out, in_), reads=inbs, writes=[outb])

    def memset(self, e, ap, val, outb):
        return self.op(e, lambda v: v.memset(ap, val), reads=(), writes=[outb])


def _rr(lst):
    i = [0]

    def nxt():
        b = lst[i[0] % len(lst)]
        i[0] += 1
        return b
    return nxt


def build(debug=None):
    k = KB()
    nc = k.nc
    es = k.root
    dbg = debug is not None
    skind = "ExternalOutput" if dbg else "Internal"

    def din(name, shape, dt=F32):
        return nc.dram_tensor(name, shape, dt, kind="ExternalInput").ap()

    def dsc(name, shape, dt):
        return nc.dram_tensor(name, shape, dt, kind=skind).ap()

    x_d = din("x", [S, D])
    c_d = din("c", [1, D])
    pos_d = din("positions", [1, S], I32)
    cst_d = din("cst", [128, 4])
    w_ada_d = din("w_ada", [D, 6 * D])
    b_ada_d = din("b_ada", [1, 6 * D])
    w_in_d = din("w_in", [D, 4256])
    gq_d = din("mla_q_norm_g", [1, 384])
    w_qup_d = din("w_q_up", [384, 768])
    gkv_d = din("mla_kv_norm_g", [1, 256])
    w_kvup_d = din("w_kv_up", [256, 1024])
    w_bsb_d = din("w_branch_sb", [512, D])
    w_bmla_d = din("w_branch_mla", [512, D])
    w_out_d = din("w_out", [D, D])
    ln1g_d = din("ln1_g", [1, D])
    ln1b_d = din("ln1_b", [1, D])
    w_rg_d = din("w_router_group", [D, 4])
    b_rg_d = din("b_router_group", [1, 4])
    w_re_d = din("w_router_expert", [D, 32])
    b_re_d = din("b_router_expert", [1, 32])
    NE_DECL = 1 if (dbg and debug != "C") else 32
    w_eg_d = din("w_exp_gate", [NE_DECL, D, 256])
    w_eu_d = din("w_exp_up", [NE_DECL, D, 256])
    w_ed_d = din("w_exp_down", [NE_DECL, 256, D])
    ln2g_d = din("ln2_g", [1, D])
    ln2b_d = din("ln2_b", [1, D])
    out_d = nc.dram_tensor("out", [S, D], F32, kind="ExternalOutput").ap()

    mod_d = dsc("mod_s", [1, 6 * D], F32)
    cos_d = dsc("cos_s", [96, S], F32)
    sin_d = dsc("sin_s", [96, S], F32)
    qsb_d = dsc("qsb_s", [4, 128, S], BF16)
    ksb_d = dsc("ksb_s", [4, 128, S], BF16)
    vsb_d = dsc("vsb_s", [S, 512], BF16)
    qm_d = dsc("qm_s", [8, 96, S], BF16)
    km_d = dsc("km_s", [8, 96, S], BF16)
    vm_d = dsc("vm_s", [S, 640], BF16)
    o_d = dsc("o_s", [S, 1024], BF16)
    acc_d = dsc("acc_s", [S, D], F32)
    u2T_d = dsc("u2T_s", [8, 128, S], BF16)
    comb_d = dsc("comb_s", [S, 32], F32)

    st = contextlib.ExitStack()
    st.enter_context(nc.allow_non_contiguous_dma(reason="small strided setup loads"))

    identF = k.sb(es, [128, 128], F32, "identF")
    identB = k.sb(es, [128, 128], BF16, "identB")
    onesB = k.sb(es, [128, 128], BF16, "onesB")
    modT = k.sb(es, [128, 48], F32, "modT")
    sc1p = k.sb(es, [128, 8], F32, "sc1p")
    cst = k.sb(es, [128, 4], F32, "cst")

    k.memset("pool", identF[:, :], 1.0, identF)
    k.op("pool", lambda g: g.affine_select(out=identF[:, :], in_=identF[:, :], pattern=[[-1, 128]],
                                           compare_op=ALU.is_equal, fill=0.0, base=0, channel_multiplier=1),
         reads=(), writes=[identF])
    k.cp("dve", identB[:, :], identF[:, :], identB, [identF])
    k.memset("dve", onesB[:, :], 1.0, onesB)
    k.dma("sp", cst[:, :], cst_d[:, :], writes=[cst])

    with contextlib.ExitStack() as ph:
        k.new_phase()
        cT = k.sb(ph, [128, 8], F32, "cT")
        cA = k.sb(ph, [128, 8], F32, "cA")
        bada = k.sb(ph, [1, 6 * D], F32, "bada")
        modrow = k.sb(ph, [1, 6 * D], F32, "modrow")
        wa = [k.sb(ph, [128, 8, 512], F32, "wa") for _ in range(2)]
        pm = [k.ps(ph, [128, 512], F32, "pm") for _ in range(2)]
        mod_db = Buf(None, "mod_d")
        k.dma("sp", cT[:, :], c_d.rearrange("o (c p) -> p (o c)", p=128), writes=[cT])
        k.dma("sp", bada[:, :], b_ada_d[:, :], writes=[bada])
        k.act(cA[:, :], cT[:, :], AF.Silu, cA, [cT])
        wv = w_ada_d.rearrange("(c p) n -> p c n", p=128)
        for nb in range(12):
            w = wa[nb % 2]
            k.dma("sp", w[:, :, :], wv[:, :, nb * 512:(nb + 1) * 512], writes=[w])
            p = pm[nb % 2]
            k.mm(p[0:1, :], [(cA[:, c:c + 1], w[:, c, :]) for c in range(8)], p, [cA, w])
            k.tt("dve", modrow[0:1, nb * 512:(nb + 1) * 512], p[0:1, :], bada[0:1, nb * 512:(nb + 1) * 512],
                 ALU.add, modrow, [p, bada])
        k.dma("sp", mod_d[:, :], modrow[:, :], reads=[modrow], writes=[mod_db])
        k.dma("sp", modT[:, :], mod_d.rearrange("o (j p) -> p (o j)", p=128), reads=[mod_db], writes=[modT])
        k.ts(sc1p[:, :], modT[:, 8:16], 1.0, None, ALU.add, None, sc1p, [modT])

        posi = k.sb(ph, [96, S], I32, "posi")
        ang = k.sb(ph, [96, S], F32, "ang")
        t0 = k.sb(ph, [96, S], F32, "t0")
        t1 = k.sb(ph, [96, S], F32, "t1")
        k.dma("sp", posi[:, :], pos_d[0:1, :].partition_broadcast(96), writes=[posi])
        k.cp("pool", ang[:, :], posi[:, :], ang, [posi])
        k.ts(ang[:, :], ang[:, :], cst[0:96, 0:1], None, ALU.mult, None, ang, [ang, cst])
        for which in range(2):
            src = ang
            if which == 1:
                k.ts(t1[:, :], ang[:, :], TWO_PI / 4.0, None, ALU.add, None, t1, [ang])
                src = t1
            k.ts(t0[:, :], src[:, :], 1.0 / TWO_PI, MAGIC, ALU.mult, ALU.add, t0, [src])
            k.ts(t0[:, :], t0[:, :], -MAGIC, -TWO_PI, ALU.add, ALU.mult, t0, [t0])
            k.tt("dve", t0[:, :], t0[:, :], src[:, :], ALU.add, t0, [t0, src])
            k.ts(t0[:, :], t0[:, :], -3.1415925, 3.1415925, ALU.max, ALU.min, t0, [t0])
            k.act(t0[:, :], t0[:, :], AF.Sin, t0, [t0])
            if which == 0:
                k.ts(t0[:, :], t0[:, :], cst[0:96, 1:2], None, ALU.mult, None, t0, [t0, cst])
                k.dma("sp", sin_d[:, :], t0[:, :], reads=[t0])
            else:
                k.dma("sp", cos_d[:, :], t0[:, :], reads=[t0])
        k.barrier()
    if debug == "0":
        return k, st

    with contextlib.ExitStack() as ph:
        k.new_phase()
        NCA = 2208
        Win = k.sb(ph, [128, 8, NCA], BF16, "Win")
        WkrE = k.sb(ph, [128, 8, 96], BF16, "WkrE")
        WkrS = k.sb(ph, [128, 8, 96], BF16, "WkrS")
        WqP = k.sb(ph, [128, 3, 768], BF16, "WqP")
        WqS = k.sb(ph, [128, 3, 768], BF16, "WqS")
        WknE = k.sb(ph, [128, 2, 768], BF16, "WknE")
        Wv = k.sb(ph, [128, 2, 512], BF16, "Wv")
        wsub = contextlib.ExitStack()
        gqT = k.sb(wsub, [128, 3], F32, "gqT")
        gkvT = k.sb(wsub, [128, 2], F32, "gkvT")
        wst = k.sb(wsub, [128, 3, 1024], F32, "wst")
        wvin = w_in_d.rearrange("(c p) n -> p c n", p=128)
        for c in range(8):
            k.dma("pool", Win[:, c, :], wvin[:, c, 0:NCA], writes=[Win])
        k.memset("dve", WkrE[:, :, :], 0.0, WkrE)
        k.memset("dve", WkrS[:, :, :], 0.0, WkrS)
        k.cp("dve", WkrE[:, :, 64:96], Win[:, :, C_KR:C_KR + 32], WkrE, [Win])
        k.cp("dve", WkrS[:, :, 64:80], Win[:, :, C_KR + 16:C_KR + 32], WkrS, [Win])
        k.cp("dve", WkrS[:, :, 80:96], Win[:, :, C_KR:C_KR + 16], WkrS, [Win])
        k.dma("sp", gqT[:, :], gq_d.rearrange("o (c p) -> p (o c)", p=128), writes=[gqT])
        k.dma("sp", gkvT[:, :], gkv_d.rearrange("o (c p) -> p (o c)", p=128), writes=[gkvT])
        k.dma("sp", wst[:, :, 0:768], w_qup_d.rearrange("(c p) n -> p c n", p=128), writes=[wst])
        for c in range(3):
            k.ts(WqP[:, c, :], wst[:, c, 0:768], gqT[:, c:c + 1], None, ALU.mult, None, WqP, [wst, gqT])
        k.memset("pool", WqS[:, :, :], 0.0, WqS)
        wqp4 = WqP.t.rearrange("p c (h e) -> p c h e", e=96)
        wqs4 = WqS.t.rearrange("p c (h e) -> p c h e", e=96)
        for c in range(3):
            k.cp("pool", wqs4[:, c, :, 64:80], wqp4[:, c, :, 80:96], WqS, [WqP])
            k.cp("pool", wqs4[:, c, :, 80:96], wqp4[:, c, :, 64:80], WqS, [WqP])
        k.dma("sp", wst[:, 0:2, :], w_kvup_d.rearrange("(c p) n -> p c n", p=128), writes=[wst])
        k.memset("pool", WknE[:, :, :], 0.0, WknE)
        wkn4 = WknE.t.rearrange("p c (h e) -> p c h e", e=96)
        wv4 = Wv.t.rearrange("p c (h e) -> p c h e", e=64)
        for c in range(2):
            w4 = wst.t[:, c, :].rearrange("p (h e) -> p h e", e=128)
            k.ts(wkn4[:, c, :, 0:64], w4[:, :, 0:64], gkvT[:, c:c + 1], None, ALU.mult, None, WknE, [wst, gkvT])
            k.ts(wv4[:, c, :, :], w4[:, :, 64:128], gkvT[:, c:c + 1], None, ALU.mult, None, Wv, [wst, gkvT])

        k.barrier()
        wsub.close()
        xg = [k.sb(ph, [128, 4, D], F32, "xg") for _ in range(2)]
        uT = [k.sb(ph, [128, 8, 512], BF16, "uT") for _ in range(2)]
        qk_out = [k.sb(ph, [128, 8, 512], BF16, "qk_out") for _ in range(2)]
        vs_out = [k.sb(ph, [128, 4, 512], BF16, "vs_out") for _ in range(2)]
        latT = k.sb(ph, [128, 5, 512], BF16, "latT")
        sq = k.sb(ph, [128, 5, 512], BF16, "sq")
        cosg = [k.sb(ph, [96, 512], F32, "cosg") for _ in range(2)]
        sing = [k.sb(ph, [96, 512], F32, "sing") for _ in range(2)]
        krope = k.sb(ph, [96, 512], F32, "krope")
        sqrq = k.sb(ph, [128, 512], F32, "sqrq")
        sqrkv = k.sb(ph, [128, 512], F32, "sqrkv")
        rq = k.sb(ph, [128, 512], F32, "rq")
        rkv = k.sb(ph, [128, 512], F32, "rkv")
        cosR = k.sb(ph, [96, 512], F32, "cosR")
        sinR = k.sb(ph, [96, 512], F32, "sinR")
        tA = [k.sb(ph, [96, 512], F32, "tA") for _ in range(2)]
        tB = [k.sb(ph, [96, 512], F32, "tB") for _ in range(2)]
        tC = [k.sb(ph, [96, 512], F32, "tC") for _ in range(2)]
        qm_out = [k.sb(ph, [96, 8, 512], BF16, "qm_out") for _ in range(1)]
        km_out = [k.sb(ph, [96, 8, 512], BF16, "km_out") for _ in range(1)]
        vm_out = [k.sb(ph, [128, 4, 640], BF16, "vm_out") for _ in range(2)]
        cols = [k.sb(ph, [128, 2], F32, "cols") for _ in range(2)]
        banks = [k.ps(ph, [128, 512], F32, "bk") for _ in range(8)]
        nb_ = _rr(banks)
        for v in vm_out:
            k.memset("pool", v[:, :, :], 1.0, v)
        xv = x_d.rearrange("(t p) d -> p t d", p=128)
        k.dma("sp", xg[0][:, :, :], xv[:, 0:4, :], writes=[xg[0]])
        for g in range(NG):
            X = xg[g % 2]
            U = uT[g % 2]
            if g + 1 < NG:
                k.dma("sp", xg[(g + 1) % 2][:, :, :], xv[:, 4 * (g + 1):4 * (g + 2), :], writes=[xg[(g + 1) % 2]])
            cg, sg = cosg[g % 2], sing[g % 2]
            k.dma("sp", cg[:, :], cos_d[:, g * 512:(g + 1) * 512], writes=[cg])
            k.dma("sp", sg[:, :], sin_d[:, g * 512:(g + 1) * 512], writes=[sg])
            for c in range(8):
                b = nb_()
                for t in range(4):
                    k.tr(b[:, t * 128:(t + 1) * 128], X[:, t, c * 128:(c + 1) * 128], identF[:, :], b, [X, identF],
                         signal=(t == 3))
                k.act(U[:, c, :], b[:, :], AF.Identity, U, [b, sc1p, modT], scale=sc1p[:, c:c + 1], bias=modT[:, c:c + 1])
            QK = qk_out[g % 2]
            for blk in range(8):
                b = nb_()
                k.mm(b[:, :], [(Win[:, c, blk * 128:(blk + 1) * 128], U[:, c, :]) for c in range(8)], b, [Win, U])
                k.cp("act" if blk % 2 == 0 else "dve", QK[:, blk, :], b[:, :], QK, [b])
            k.dma("sp", qsb_d[:, :, g * 512:(g + 1) * 512].rearrange("a p t -> p a t"), QK[:, 0:4, :], reads=[QK])
            k.dma("sp", ksb_d[:, :, g * 512:(g + 1) * 512].rearrange("a p t -> p a t"), QK[:, 4:8, :], reads=[QK])
            VS = vs_out[g % 2]
            for t in range(4):
                b = nb_()
                k.mm(b[:, :], [(U[:, c, t * 128:(t + 1) * 128], Win[:, c, C_VSB:C_VSB + 512]) for c in range(8)], b, [Win, U])
                k.cp("dve", VS[:, t, :], b[:, :], VS, [b])
            k.dma("sp", vsb_d[g * 512:(g + 1) * 512, :].rearrange("(t p) n -> p t n", p=128), VS[:, :, :], reads=[VS])
            for i in range(5):
                b = nb_()
                c0 = C_QD + i * 128
                k.mm(b[:, :], [(Win[:, c, c0:c0 + 128], U[:, c, :]) for c in range(8)], b, [Win, U])
                k.cp("act", latT[:, i, :], b[:, :], latT, [b])
                k.act(sq[:, i, :], b[:, :], AF.Square, sq, [b])
            b1 = nb_()
            k.mm(b1[0:96, :], [(WkrE[:, c, :], U[:, c, :]) for c in range(8)], b1, [WkrE, U])
            b2 = nb_()
            k.mm(b2[0:96, :], [(WkrS[:, c, :], U[:, c, :]) for c in range(8)], b2, [WkrS, U])
            k.tt("dve", tA[0][:, :], b1[0:96, :], cg[:, :], ALU.mult, tA[0], [b1, cg])
            k.tt("dve", tB[0][:, :], b2[0:96, :], sg[:, :], ALU.mult, tB[0], [b2, sg])
            k.tt("pool", krope[:, :], tA[0][:, :], tB[0][:, :], ALU.add, krope, [tA[0], tB[0]])
            bq = nb_()
            k.mm(bq[:, :], [(onesB[:, :], sq[:, i, :]) for i in range(3)], bq, [onesB, sq])
            k.act(sqrq[:, :], bq[:, :], AF.Sqrt, sqrq, [bq], scale=1.0 / 384.0, bias=1e-6)
            k.op("dve", lambda v: v.reciprocal(rq[:, :], sqrq[:, :]), reads=[sqrq], writes=[rq])
            bkv = nb_()
            k.mm(bkv[:, :], [(onesB[:, :], sq[:, 3 + i, :]) for i in range(2)], bkv, [onesB, sq])
            k.act(sqrkv[:, :], bkv[:, :], AF.Sqrt, sqrkv, [bkv], scale=1.0 / 256.0, bias=1e-6)
            k.op("dve", lambda v: v.reciprocal(rkv[:, :], sqrkv[:, :]), reads=[sqrkv], writes=[rkv])
            k.tt("pool", cosR[:, :], cg[:, :], rq[0:96, :], ALU.mult, cosR, [cg, rq])
            k.tt("pool", sinR[:, :], sg[:, :], rq[0:96, :], ALU.mult, sinR, [sg, rq])
            VM = vm_out[g % 2]
            vm4 = VM.t.rearrange("p t (h e) -> p t h e", e=80)
            for t in range(4):
                co = cols[t % 2]
                b = nb_()
                k.mm(b[:, 0:1], [(sq[:, 3 + i, t * 128:(t + 1) * 128], onesB[:, 0:1]) for i in range(2)], b, [sq, onesB])
                k.act(co[:, 0:1], b[:, 0:1], AF.Sqrt, co, [b], scale=1.0 / 256.0, bias=1e-6)
                k.op("dve", lambda v, co=co: v.reciprocal(co[:, 1:2], co[:, 0:1]), reads=[co], writes=[co])
                b = nb_()
                k.mm(b[:, :], [(latT[:, 3 + i, t * 128:(t + 1) * 128], Wv[:, i, :]) for i in range(2)], b, [latT, Wv])
                k.ts(vm4[:, t, :, 0:64], b.t[:, :].rearrange("p (h e) -> p h e", e=64), co[:, 1:2], None, ALU.mult, None,
                     VM, [b, co])
            k.dma("sp", vm_d[g * 512:(g + 1) * 512, :].rearrange("(t p) n -> p t n", p=128), VM[:, :, :], reads=[VM])
            QM = qm_out[0]
            KM = km_out[0]
            for h in range(8):
                a, bb_, cc = tA[h % 2], tB[h % 2], tC[h % 2]
                b1 = nb_()
                k.mm(b1[0:96, :], [(WqP[:, i, h * 96:(h + 1) * 96], latT[:, i, :]) for i in range(3)], b1, [WqP, latT])
                b2 = nb_()
                k.mm(b2[0:96, :], [(WqS[:, i, h * 96:(h + 1) * 96], latT[:, i, :]) for i in range(3)], b2, [WqS, latT])
                k.tt("dve", a[:, :], b1[0:96, :], cosR[:, :], ALU.mult, a, [b1, cosR])
                k.tt("dve", bb_[:, :], b2[0:96, :], sinR[:, :], ALU.mult, bb_, [b2, sinR])
                k.tt("pool", QM[:, h, :], a[:, :], bb_[:, :], ALU.add, QM, [a, bb_])
                b3 = nb_()
                k.mm(b3[0:96, :], [(WknE[:, i, h * 96:(h + 1) * 96], latT[:, 3 + i, :]) for i in range(2)], b3, [WknE, latT])
                k.tt("dve", cc[:, :], b3[0:96, :], rkv[0:96, :], ALU.mult, cc, [b3, rkv])
                k.tt("pool", KM[:, h, :], cc[:, :], krope[:, :], ALU.add, KM, [cc, krope])
            k.dma("sp", qm_d[:, :, g * 512:(g + 1) * 512].rearrange("h p t -> p h t"), QM[:, :, :], reads=[QM])
            k.dma("sp", km_d[:, :, g * 512:(g + 1) * 512].rearrange("h p t -> p h t"), KM[:, :, :], reads=[KM])
        k.barrier()
    if debug == "A":
        return k, st

    with contextlib.ExitStack() as ph:
        k.new_phase()
        kT = [k.sb(ph, [128, S], BF16, "kTsb") for _ in range(4)]
        Vs = [k.sb(ph, [128, 8, 512], BF16, "Vsb") for _ in range(4)]
        for p in range(4):
            k.dma("sp", kT[p][:, :], ksb_d[p, :, :], writes=[kT[p]])
        vv = vsb_d.rearrange("(t p) n -> p t n", p=128)
        for i in range(4):
            k.dma("sp", Vs[i][:, :, :], vv[:, 8 * i:8 * i + 8, :], writes=[Vs[i]])
        onesF = k.sb(ph, [128, 1024], F32, "onesF")
        maskS = k.sb(ph, [128, 128], BF16, "maskS")
        mtmp = k.sb(ph, [128, 128], F32, "mtmp")
        k.memset("dve", onesF[:, :], 1e-30, onesF)
        k.memset("pool", mtmp[:, :], 0.0, mtmp)
        k.op("pool", lambda g_: g_.affine_select(out=mtmp[:, :], in_=mtmp[:, :], pattern=[[1, 128]], compare_op=ALU.is_gt,
                                                 fill=NEG_BIG, base=0, channel_multiplier=-1), reads=(), writes=[mtmp])
        k.cp("dve", maskS[:, :], mtmp[:, :], maskS, [mtmp])
        qg = [k.sb(ph, [128, 4, 512], BF16, "qg") for _ in range(2)]
        om = [k.sb(ph, [128, 1024], F32, "om") for _ in range(3)]
        cpb = [k.sb(ph, [128, 1032], F32, "cpb") for _ in range(3)]
        wb = [k.sb(ph, [128, 1024], BF16, "wb") for _ in range(3)]
        wT = [k.sb(ph, [128, 1024], BF16, "wT") for _ in range(3)]
        oout = [k.sb(ph, [128, 4, 512], BF16, "oout") for _ in range(2)]
        zb = [k.ps(ph, [128, 1024], F32, "zb") for _ in range(2)]
        wTp = [k.ps(ph, [128, 1024], BF16, "wTp") for _ in range(2)]
        oslots = [k.ps(ph, [128, 64], F32, "oslot") for i in range(2)]
        chunks = []
        for g in range(NG):
            for h in range(8):
                for j in range(4):
                    qb = 4 * g + j
                    nkb = 32 - qb
                    nch = (nkb + 7) // 8
                    for ci in range(nch):
                        kb0 = qb + 8 * ci
                        nb = min(8, 32 - kb0)
                        chunks.append((g, h, j, ci, nch, kb0, nb))
        if LIMIT is not None:
            chunks = chunks[START:LIMIT]
        N = len(chunks)
        slot_of = {}
        sl = [0]

        def S1(n):
            g, h, j, ci, nch, kb0, nb = chunks[n]
            p, a = h // 2, h % 2
            W = nb * 128
            Q = qg[g % 2]
            if (h == 0 and j == 0 and ci == 0) or n == 0:
                k.dma("sp", Q[:, :, :], qsb_d[:, :, g * 512:(g + 1) * 512].rearrange("a p t -> p a t"), writes=[Q])
            z = zb[n % 2]
            lq = Q[a * 64:(a + 1) * 64, p, j * 128:(j + 1) * 128]
            kt = kT[p]
            if ci == 0:
                k.op("pe", lambda pe: pe.matmul(z[:, 0:128], lhsT=lq, rhs=kt[a * 64:(a + 1) * 64, kb0 * 128:kb0 * 128 + 128],
                                                 start=True, stop=False), reads=[Q, kt], writes=[z], signal=False)
                k.op("pe", lambda pe: pe.matmul(z[:, 0:128], lhsT=identB[:, :], rhs=maskS[:, :], start=False, stop=True),
                     reads=[identB, maskS], writes=[], signal=(W == 128))
                segs = [(c0, min(c0 + 512 - (c0 % 512), W)) for c0 in ([128] if W > 128 else []) + ([512] if W > 512 else [])]
                for si_, (c0, c1) in enumerate(segs):
                    k.op("pe", lambda pe, c0=c0, c1=c1: pe.matmul(z[:, c0:c1], lhsT=lq,
                                                                 rhs=kt[a * 64:(a + 1) * 64, kb0 * 128 + c0:kb0 * 128 + c1],
                                                                 start=True, stop=True), reads=[], writes=[], signal=(si_ == len(segs) - 1))
            else:
                segs = [(0, min(512, W))] + ([(512, W)] if W > 512 else [])
                for si_, (c0, c1) in enumerate(segs):
                    k.op("pe", lambda pe, c0=c0, c1=c1: pe.matmul(z[:, c0:c1], lhsT=lq,
                                                                 rhs=kt[a * 64:(a + 1) * 64, kb0 * 128 + c0:kb0 * 128 + c1],
                                                                 start=True, stop=True),
                         reads=[Q, kt] if si_ == 0 else [], writes=[z] if si_ == 0 else [], signal=(si_ == len(segs) - 1))
            o_ = om[n % 3]
            k.act(o_[:, 0:W], z[:, 0:W], AF.Sigmoid, o_, [z], scale=-0.125)
            c_ = cpb[n % 3]
            if ci == 0:
                k.memset("dve", c_[:, 0:1], 1.0, c_)
            else:
                cprev = cpb[(n - 1) % 3]
                k.cp("dve", c_[:, 0:1], cprev[:, 1024:1025], c_, [cprev])
            k.op("dve", lambda v: v.tensor_tensor_scan(out=c_[:, 1:1 + W], data0=o_[:, 0:W], data1=onesF[:, 0:W],
                                                       initial=c_[:, 0:1], op0=ALU.mult, op1=ALU.max),
                 reads=[o_, onesF], writes=[c_])
            w_ = wb[n % 3]
            k.tt("pool", w_[:, 0:W], c_[:, 0:W], c_[:, 1:1 + W], ALU.subtract, w_, [c_])

        def S2(n):
            g, h, j, ci, nch, kb0, nb = chunks[n]
            W = nb * 128
            w_ = wb[n % 3]
            tp = wTp[n % 2]
            for i in range(nb):
                k.tr(tp[:, i * 128:(i + 1) * 128], w_[:, i * 128:(i + 1) * 128], identB[:, :], tp, [w_, identB],
                     signal=(i == nb - 1))
            k.cp("act" if n % 2 == 0 else "dve", wT[n % 3][:, 0:W], tp[:, 0:W], wT[n % 3], [tp])

        def S3(n):
            g, h, j, ci, nch, kb0, nb = chunks[n]
            if ci == 0:
                slot_of[(g, h, j)] = sl[0] % 2
                sl[0] += 1
            si = slot_of[(g, h, j)]
            osl = oslots[si]
            oap = osl[:, :]
            t_ = wT[n % 3]
            for i in range(nb):
                kb = kb0 + i
                vb = Vs[kb // 8]
                first = (ci == 0 and i == 0)
                last = (ci == nch - 1 and i == nb - 1)
                k.op("pe", lambda pe, i=i, kb=kb, vb=vb, first=first, last=last:
                     pe.matmul(oap, lhsT=t_[:, i * 128:(i + 1) * 128], rhs=vb[:, kb % 8, h * 64:(h + 1) * 64],
                               start=first, stop=last),
                     reads=[t_, vb] if i == 0 else [vb], writes=[osl] if i == 0 else [], signal=(i == nb - 1))
            if ci == nch - 1:
                O = oout[g % 2]
                k.cp("dve", O[:, j, h * 64:(h + 1) * 64], oap, O, [osl])
                if h == 7 and j == 3:
                    for tt_ in range(4):
                        r0 = g * 512 + tt_ * 128
                        k.dma("sp", o_d[r0:r0 + 128, 0:512], O[:, tt_, :], reads=[O])

        for n in range(N + SK3):
            if n < N:
                S1(n)
            if 0 <= n - SK2 < N:
                S2(n - SK2)
            if 0 <= n - SK3 < N:
                S3(n - SK3)
        for _ in range(PAD):
            k.memset("dve", mtmp[:, 0:8], 0.0, mtmp)
        k.barrier()
    if debug == "B1s":
        return k, st

    with contextlib.ExitStack() as ph:
        k.new_phase()
        kM = [k.sb(ph, [96, S], BF16, "kM") for _ in range(8)]
        Vm = [k.sb(ph, [128, 8, 640], BF16, "Vm") for _ in range(4)]
        for h in range(8):
            k.dma("sp", kM[h][:, :], km_d[h, :, :], writes=[kM[h]])
        vv = vm_d.rearrange("(t p) n -> p t n", p=128)
        for i in range(4):
            k.dma("sp", Vm[i][:, :, :], vv[:, 8 * i:8 * i + 8, :], writes=[Vm[i]])
        maskM = k.sb(ph, [128, 128], BF16, "maskM")
        zB = k.sb(ph, [128, 128], BF16, "zB")
        k.memset("dve", zB[:, :], 0.0, zB)
        mtmp = k.sb(ph, [128, 128], F32, "mtmp2")
        k.memset("pool", mtmp[:, :], 1.0, mtmp)
        k.op("pool", lambda g_: g_.affine_select(out=mtmp[:, :], in_=mtmp[:, :], pattern=[[-1, 128]], compare_op=ALU.is_gt,
                                                 fill=0.0, base=1, channel_multiplier=1), reads=(), writes=[mtmp])
        k.cp("dve", maskM[:, :], mtmp[:, :], maskM, [mtmp])
        qm = [k.sb(ph, [96, 8, 512], BF16, "qm") for _ in range(2)]
        pT = [k.sb(ph, [128, 512], BF16, "pT") for _ in range(6)]
        oout = [k.sb(ph, [128, 4, 512], BF16, "ooutm") for _ in range(2)]
        rc = [k.sb(ph, [128, 8], F32, "rc") for _ in range(2)]
        sb_ = [k.ps(ph, [128, 512], F32, "sTb") for _ in range(3)]
        oM = [k.ps(ph, [128, 512], F32, "oM") for _ in range(2)]
        SC = 1.0 / (96.0 ** 0.5)
        blocks = []
        for g in range(NG):
            for h in range(8):
                for kb in range(4 * g, 32):
                    blocks.append((g, h, kb))
        if MLIMIT is not None:
            blocks = blocks[:MLIMIT]
        N = len(blocks)

        def M1(n):
            g, h, kb = blocks[n]
            m = kb - 4 * g
            Q = qm[g % 2]
            if h == 0 and m == 0:
                k.dma("sp", Q[:, :, :], qm_d[:, :, g * 512:(g + 1) * 512].rearrange("h p t -> p h t"), writes=[Q])
            s_ = sb_[n % 3]
            kk = kM[h]
            lk = kk[:, kb * 128:(kb + 1) * 128]
            if m >= 4:
                ncol = 512
                k.op("pe", lambda pe: pe.matmul(s_[:, 0:512], lhsT=lk, rhs=Q[:, h, :], start=True, stop=True),
                     reads=[kk, Q], writes=[s_], signal=True)
            else:
                ncol = (m + 1) * 128
                if m > 0:
                    k.op("pe", lambda pe: pe.matmul(s_[:, 0:m * 128], lhsT=lk, rhs=Q[:, h, 0:m * 128], start=True, stop=True),
                         reads=[kk, Q], writes=[s_], signal=False)
                k.op("pe", lambda pe: pe.matmul(s_[:, m * 128:ncol], lhsT=lk, rhs=Q[:, h, m * 128:ncol], start=True, stop=True),
                     reads=[kk, Q], writes=[s_], signal=True)
            k.act(pT[n % 6][:, 0:ncol], s_[:, 0:ncol], AF.Exp, pT[n % 6], [s_], scale=SC)
            if m < 4:
                p__ = pT[n % 6]
                k.tt("dve", p__[:, m * 128:ncol], p__[:, m * 128:ncol], maskM[:, :], ALU.mult, p__, [p__, maskM])

        def M2(n):
            g, h, kb = blocks[n]
            m = kb - 4 * g
            acc = oM[(g * 8 + h) % 2]
            acc4 = acc.t[:, 0:320].rearrange("p (j e) -> p j e", e=80)
            p_ = pT[n % 6]
            vb = Vm[kb // 8]
            vap = vb.t[:, kb % 8, :].rearrange("p (hh e) -> p hh e", e=80)
            nj = min(m, 3) + 1
            if m == 0:
                k.op("pe", lambda pe: pe.matmul(acc.t[:, 0:320], lhsT=zB[:, :], rhs=Vm[0].t[:, 0, 0:320], start=True, stop=False),
                     reads=[zB, Vm[0]], writes=[acc], signal=False)
            for j in range(nj):
                first = False
                last = (kb == 31)
                k.op("pe", lambda pe, j=j, first=first, last=last:
                     pe.matmul(acc4[:, j, 0:66], lhsT=p_[:, j * 128:(j + 1) * 128], rhs=vap[:, h, 0:66], start=first, stop=last),
                     reads=[p_, vb] if j == 0 else [], writes=[acc] if j == 0 else [], signal=(j == nj - 1))
            if kb == 31 and not NOFIN:
                O = oout[g % 2]
                r_ = rc[(g * 8 + h) % 2]
                for j in range(4):
                    k.cp("dve", r_[:, 4 + j:5 + j], acc4[:, j, 64:65], r_, [acc])
                if not DBGM:
                    k.op("dve", lambda v: v.reciprocal(r_[:, 0:4], r_[:, 4:8]), reads=[r_], writes=[r_])
                for j in range(4):
                    if DBGM:
                        k.cp("dve", O[:, j, h * 64:(h + 1) * 64], acc4[:, j, 1:65], O, [acc])
                    else:
                        k.ts(O[:, j, h * 64:(h + 1) * 64], acc4[:, j, 0:64], r_[:, j:j + 1], None, ALU.mult, None, O, [acc, r_])
                if DBGM and MLIMIT is not None and h == 1:
                    for tt_ in range(4):
                        r0 = g * 512 + tt_ * 128
                        k.dma("sp", o_d[r0:r0 + 128, 512:1024], O[:, tt_, :], reads=[O])
                if h == 7:
                    for tt_ in range(4):
                        r0 = g * 512 + tt_ * 128
                        k.dma("sp", o_d[r0:r0 + 128, 512:1024], O[:, tt_, :], reads=[O])

        for n in range(N + 4):
            if n < N:
                M1(n)
            if 0 <= n - 4 < N:
                M2(n - 4)
        k.barrier()
    if debug == "B1":
        return k, st

    with contextlib.ExitStack() as ph:
        k.new_phase()
        wvin = w_in_d.rearrange("(c p) n -> p c n", p=128)
        Wgi = k.sb(ph, [128, 8, 2048], BF16, "Wgi")
        for c in range(8):
            k.dma("pool", Wgi[:, c, :], wvin[:, c, C_GSB:C_GSB + 2048], writes=[Wgi])
        Wb = k.sb(ph, [128, 8, D], BF16, "Wb")
        k.dma("pool", Wb[:, 0:4, :], w_bsb_d.rearrange("(c p) n -> p c n", p=128), writes=[Wb])
        k.dma("pool", Wb[:, 4:8, :], w_bmla_d.rearrange("(c p) n -> p c n", p=128), writes=[Wb])
        Wo = k.sb(ph, [128, 8, D], BF16, "Wo")
        k.dma("pool", Wo[:, :, :], w_out_d.rearrange("(c p) n -> p c n", p=128), writes=[Wo])
        Wr = k.sb(ph, [128, 8, 36], F32, "Wr")
        k.dma("sp", Wr[:, :, 0:4], w_rg_d.rearrange("(c p) n -> p c n", p=128), writes=[Wr])
        k.dma("sp", Wr[:, :, 4:36], w_re_d.rearrange("(c p) n -> p c n", p=128), writes=[Wr])
        brow = k.sb(ph, [128, 36], F32, "brow")
        k.dma("sp", brow[:, 0:4], b_rg_d[0:1, :].partition_broadcast(128), writes=[brow])
        k.dma("sp", brow[:, 4:36], b_re_d[0:1, :].partition_broadcast(128), writes=[brow])
        g1bc = k.sb(ph, [128, D], F32, "g1bc")
        k.dma("sp", g1bc[:, :], mod_d[0:1, 2 * D:3 * D].partition_broadcast(128), writes=[g1bc])
        gA = k.sb(ph, [128, D], F32, "gA")
        bA = k.sb(ph, [128, D], F32, "bA")
        k.dma("sp", gA[:, :], ln1g_d[0:1, :].partition_broadcast(128), writes=[gA])
        k.dma("sp", bA[:, :], ln1b_d[0:1, :].partition_broadcast(128), writes=[bA])
        k.ts(gA[:, :], gA[:, :], ALPHA, None, ALU.mult, None, gA, [gA])
        k.ts(bA[:, :], bA[:, :], ALPHA, None, ALU.mult, None, bA, [bA])
        lgT = k.sb(ph, [128, 8], F32, "lgT")
        lbT = k.sb(ph, [128, 8], F32, "lbT")
        A2 = k.sb(ph, [128, 8], F32, "A2")
        B2 = k.sb(ph, [128, 8], F32, "B2")
        k.dma("sp", lgT[:, :], ln1g_d.rearrange("o (c p) -> p (o c)", p=128), writes=[lgT])
        k.dma("sp", lbT[:, :], ln1b_d.rearrange("o (c p) -> p (o c)", p=128), writes=[lbT])
        k.ts(A2[:, :], modT[:, 32:40], 1.0, None, ALU.add, None, A2, [modT])
        k.tt("dve", B2[:, :], lbT[:, :], A2[:, :], ALU.mult, B2, [lbT, A2])
        k.tt("dve", B2[:, :], B2[:, :], modT[:, 24:32], ALU.add, B2, [B2, modT])
        k.tt("dve", A2[:, :], A2[:, :], lgT[:, :], ALU.mult, A2, [A2, lgT])

        xg = k.sb(ph, [128, 4, D], F32, "xg2")
        og = k.sb(ph, [128, 4, D], BF16, "og")
        uT = k.sb(ph, [128, 8, 512], BF16, "uT2")
        gates = k.sb(ph, [128, 4, 2048], BF16, "gates")
        oT = [k.sb(ph, [128, D], BF16, "oT") for _ in range(2)]
        t1 = k.sb(ph, [128, D], F32, "t1")
        t2 = k.sb(ph, [128, D], F32, "t2")
        mixed = k.sb(ph, [128, D], BF16, "mixed")
        mT = [k.sb(ph, [128, D], BF16, "mT") for _ in range(2)]
        pre = k.sb(ph, [128, 4, D], F32, "pre")
        acc0 = [k.sb(ph, [128, D], F32, "acc0") for _ in range(2)]
        u2T = k.sb(ph, [128, 8, 512], F32, "u2T")
        u2Tb = k.sb(ph, [128, 8, 512], BF16, "u2Tb")
        stt = k.sb(ph, [128, 4, 12], F32, "stt")
        mv = k.sb(ph, [128, 4, 2], F32, "mv")
        sd = k.sb(ph, [128, 4, 2], F32, "sd")
        comb = k.sb(ph, [128, 4, 32], F32, "comb")
        rs = [k.sb(ph, [128, 160], F32, "rs") for _ in range(2)]
        fb = [k.ps(ph, [128, 512], F32, "fb") for _ in range(2)]
        tpb = [k.ps(ph, [128, D], BF16, "tpb") for _ in range(2)]
        yp = [k.ps(ph, [128, D], F32, "yp") for _ in range(2)]
        nfb = _rr(fb)
        ntp = _rr(tpb)
        xv = x_d.rearrange("(t p) d -> p t d", p=128)
        ov = o_d.rearrange("(t p) d -> p t d", p=128)
        for G in range(NG):
            k.dma("sp", xg[:, :, :], xv[:, 4 * G:4 * G + 4, :], writes=[xg])
            k.dma("sp", og[:, :, :], ov[:, 4 * G:4 * G + 4, :], writes=[og])
            for c in range(8):
                b = nfb()
                for t in range(4):
                    k.tr(b[:, t * 128:(t + 1) * 128], xg[:, t, c * 128:(c + 1) * 128], identF[:, :], b, [xg, identF], signal=(t == 3))
                k.act(uT[:, c, :], b[:, :], AF.Identity, uT, [b, sc1p, modT], scale=sc1p[:, c:c + 1], bias=modT[:, c:c + 1])
            for t in range(4):
                for blk in range(4):
                    b = nfb()
                    k.mm(b[:, :], [(uT[:, c, t * 128:(t + 1) * 128], Wgi[:, c, blk * 512:(blk + 1) * 512]) for c in range(8)], b, [uT, Wgi])
                    k.act(gates[:, t, blk * 512:(blk + 1) * 512], b[:, :], AF.Sigmoid, gates, [b])
            for t in range(4):
                tp = ntp()
                for c in range(8):
                    k.tr(tp[:, c * 128:(c + 1) * 128], og[:, t, c * 128:(c + 1) * 128], identB[:, :], tp, [og, identB], signal=(c == 7))
                o_ = oT[t % 2]
                k.cp("dve", o_[:, :], tp[:, :], o_, [tp])
                ysb, yml = yp[0], yp[1]
                for hf in range(2):
                    k.mm(ysb[:, hf * 512:(hf + 1) * 512], [(o_[:, c * 128:(c + 1) * 128], Wb[:, c, hf * 512:(hf + 1) * 512]) for c in range(4)],
                         ysb, [o_, Wb], signal=(hf == 1))
                for hf in range(2):
                    k.mm(yml[:, hf * 512:(hf + 1) * 512], [(o_[:, c * 128:(c + 1) * 128], Wb[:, c, hf * 512:(hf + 1) * 512]) for c in range(4, 8)],
                         yml, [o_, Wb], signal=(hf == 1))
                k.tt("dve", t1[:, :], ysb[:, :], gates[:, t, 0:D], ALU.mult, t1, [ysb, gates])
                k.tt("dve", t2[:, :], yml[:, :], gates[:, t, D:2 * D], ALU.mult, t2, [yml, gates])
                k.tt("pool", mixed[:, :], t1[:, :], t2[:, :], ALU.add, mixed, [t1, t2])
                tp = ntp()
                for c in range(8):
                    k.tr(tp[:, c * 128:(c + 1) * 128], mixed[:, c * 128:(c + 1) * 128], identB[:, :], tp, [mixed, identB], signal=(c == 7))
                m_ = mT[t % 2]
                k.cp("act", m_[:, :], tp[:, :], m_, [tp])
                at = ysb
                for hf in range(2):
                    k.mm(at[:, hf * 512:(hf + 1) * 512], [(m_[:, c * 128:(c + 1) * 128], Wo[:, c, hf * 512:(hf + 1) * 512]) for c in range(8)],
                         at, [m_, Wo], signal=(hf == 1))
                k.tt("dve", t1[:, :], at[:, :], g1bc[:, :], ALU.mult, t1, [at, g1bc])
                k.op("dve", lambda v, t=t: v.scalar_tensor_tensor(out=pre[:, t, :], in0=xg[:, t, :], scalar=ALPHA, in1=t1[:, :],
                                                                    op0=ALU.mult, op1=ALU.add), reads=[xg, t1], writes=[pre])
                for hf in range(2):
                    k.op("dve", lambda v, t=t, hf=hf: v.bn_stats(out=stt[:, t, hf * 6:(hf + 1) * 6], in_=pre[:, t, hf * 512:(hf + 1) * 512]),
                         reads=[pre], writes=[stt])
                k.op("dve", lambda v, t=t: v.bn_aggr(out=mv[:, t, :], in_=stt[:, t, :].rearrange("p (a b) -> p a b", b=6)),
                     reads=[stt], writes=[mv])
            for t in range(4):
                k.act(sd[:, t, 0:1], mv[:, t, 1:2], AF.Sqrt, sd, [mv], scale=1.0, bias=1e-5)
                k.op("dve", lambda v, t=t: v.reciprocal(sd[:, t, 1:2], sd[:, t, 0:1]), reads=[sd], writes=[sd])
                k.ts(pre[:, t, :], pre[:, t, :], mv[:, t, 0:1], sd[:, t, 1:2], ALU.subtract, ALU.mult, pre, [pre, mv, sd])
                a_ = acc0[t % 2]
                k.tt("pool", a_[:, :], pre[:, t, :], gA[:, :], ALU.mult, a_, [pre, gA])
                k.tt("pool", a_[:, :], a_[:, :], bA[:, :], ALU.add, a_, [a_, bA])
                k.dma("sp", acc_d[(4 * G + t) * 128:(4 * G + t + 1) * 128, :], a_[:, :], reads=[a_])
            for c in range(8):
                b = nfb()
                for t in range(4):
                    k.tr(b[:, t * 128:(t + 1) * 128], pre[:, t, c * 128:(c + 1) * 128], identF[:, :], b, [pre, identF], signal=(t == 3))
                k.act(u2T[:, c, :], b[:, :], AF.Identity, u2T, [b, A2, B2], scale=A2[:, c:c + 1], bias=B2[:, c:c + 1])
            k.cp("pool", u2Tb[:, :, :], u2T[:, :, :], u2Tb, [u2T])
            k.dma("sp", u2T_d[:, :, G * 512:(G + 1) * 512].rearrange("c p t -> p c t"), u2Tb[:, :, :], reads=[u2Tb])
            for t in range(4):
                b = nfb()
                r_ = rs[t % 2]
                k.mm(b[:, 0:36], [(u2T[:, c, t * 128:(t + 1) * 128], Wr[:, c, :]) for c in range(8)], b, [u2T, Wr])
                lg = r_[:, 0:36]
                k.tt("dve", lg, b[:, 0:36], brow[:, :], ALU.add, r_, [b, brow])
                gmax, ngm, sg4, om4, e4, ssum, pg = (r_[:, 40:41], r_[:, 41:42], r_[:, 44:48], r_[:, 48:52], r_[:, 52:56],
                                                     r_[:, 42:43], r_[:, 43:44])
                oh, elm, top8 = r_[:, 56:60], r_[:, 64:96], r_[:, 96:104]
                dd, sgd, w1, w2, c1 = r_[:, 104:105], r_[:, 105:106], r_[:, 106:107], r_[:, 107:108], r_[:, 112:144]
                R = [r_]
                k.op("dve", lambda v: v.tensor_reduce(out=gmax, in_=r_[:, 0:4], axis=AX.X, op=ALU.max), reads=R, writes=R)
                k.ts(ngm, gmax, -1.0, None, ALU.mult, None, r_, R)
                k.act(sg4, r_[:, 0:4], AF.Sigmoid, r_, R, bias=ngm)
                k.ts(om4, sg4, -1.0, 1.0, ALU.mult, ALU.add, r_, R)
                k.op("dve", lambda v: v.reciprocal(om4, om4), reads=R, writes=R)
                k.tt("dve", e4, sg4, om4, ALU.mult, r_, R)
                k.op("dve", lambda v: v.tensor_reduce(out=ssum, in_=e4, axis=AX.X, op=ALU.add), reads=R, writes=R)
                k.op("dve", lambda v: v.reciprocal(pg, ssum), reads=R, writes=R)
                k.ts(oh, r_[:, 0:4], gmax, None, ALU.is_equal, None, r_, R)
                k.ts(oh, oh, 1e30, -1e30, ALU.mult, ALU.add, r_, R)
                for gi in range(4):
                    k.ts(r_[:, 64 + 8 * gi:72 + 8 * gi], r_[:, 4 + 8 * gi:12 + 8 * gi], r_[:, 56 + gi:57 + gi], None, ALU.add, None, r_, R)
                k.op("dve", lambda v: v.max(out=top8, in_=elm), reads=R, writes=R)
                k.tt("dve", dd, r_[:, 96:97], r_[:, 97:98], ALU.subtract, r_, R)
                k.act(sgd, dd, AF.Sigmoid, r_, R)
                k.tt("dve", w1, pg, sgd, ALU.mult, r_, R)
                k.tt("dve", w2, pg, w1, ALU.subtract, r_, R)
                k.ts(c1, elm, r_[:, 96:97], w1, ALU.is_equal, ALU.mult, r_, R)
                k.ts(comb[:, t, :], elm, r_[:, 97:98], w2, ALU.is_equal, ALU.mult, comb, R)
                k.tt("dve", comb[:, t, :], comb[:, t, :], c1, ALU.add, comb, [comb, r_])
            k.dma("sp", comb_d[G * 512:(G + 1) * 512, :].rearrange("(t p) n -> p t n", p=128), comb[:, :, :], reads=[comb])
        k.barrier()
    if debug == "B2":
        return k, st

    with contextlib.ExitStack() as ph:
        k.new_phase()
        g2bc = k.sb(ph, [128, D], F32, "g2bc")
        l2g = k.sb(ph, [128, D], F32, "l2g")
        l2b = k.sb(ph, [128, D], F32, "l2b")
        k.dma("sp", g2bc[:, :], mod_d[0:1, 5 * D:6 * D].partition_broadcast(128), writes=[g2bc])
        k.dma("sp", l2g[:, :], ln2g_d[0:1, :].partition_broadcast(128), writes=[l2g])
        k.dma("sp", l2b[:, :], ln2b_d[0:1, :].partition_broadcast(128), writes=[l2b])
        acc = k.sb(ph, [128, 16, D], F32, "acc")
        u2 = k.sb(ph, [128, 8, 2048], BF16, "u2")
        cmb = k.sb(ph, [128, 16, 32], F32, "cmb")
        Wg = [k.sb(ph, [128, 8, 256], BF16, "Wg") for _ in range(2)]
        Wu = [k.sb(ph, [128, 8, 256], BF16, "Wu") for _ in range(2)]
        Wds = [k.sb(ph, [128, 2, D], F32, "Wds") for _ in range(2)]
        Wd = [k.sb(ph, [128, 2, D], BF16, "Wd") for _ in range(2)]
        sgb = [k.sb(ph, [128, 512], F32, "sgb") for _ in range(2)]
        hT = [k.sb(ph, [128, 2, 512], BF16, "hT") for _ in range(2)]
        stt = k.sb(ph, [128, 12], F32, "stt2")
        mv = k.sb(ph, [128, 16, 2], F32, "mv2")
        sd = k.sb(ph, [128, 16, 2], F32, "sd2")
        yo = [k.sb(ph, [128, D], F32, "yo") for _ in range(2)]
        gub = [k.ps(ph, [128, 512], F32, "gub") for _ in range(4)]
        dnb = [k.ps(ph, [128, D], F32, "dnb") for _ in range(2)]
        ngu = _rr(gub)
        ndn = _rr(dnb)
        NE = 32
        for ps_ in range(2):
            T0 = ps_ * 2048
            k.dma("sp", acc[:, :, :], acc_d[T0:T0 + 2048, :].rearrange("(t p) d -> p t d", p=128), writes=[acc])
            k.dma("sp", u2[:, :, :], u2T_d[:, :, T0:T0 + 2048].rearrange("c p t -> p c t"), writes=[u2])
            k.dma("sp", cmb[:, :, :], comb_d[T0:T0 + 2048, :].rearrange("(t p) n -> p t n", p=128), writes=[cmb])
            pend = None
            seq = [(e, gr) for e in range(NE) for gr in range(4)]

            def load_w(e):
                i = e % 2
                k.dma("pool", Wg[i][:, :, :], w_eg_d[e].rearrange("(c p) f -> p c f", p=128), writes=[Wg[i]])
                k.dma("pool", Wu[i][:, :, :], w_eu_d[e].rearrange("(c p) f -> p c f", p=128), writes=[Wu[i]])
                k.dma("sp", Wds[i][:, :, :], w_ed_d[e].rearrange("(c p) d -> p c d", p=128), writes=[Wds[i]])
                for c in range(2):
                    k.tt("pool", Wd[i][:, c, :], Wds[i][:, c, :], g2bc[:, :], ALU.mult, Wd[i], [Wds[i], g2bc])

            def gate_up(e, gr, it):
                i = e % 2
                H = hT[it % 2]
                for fc in range(2):
                    bg = ngu()
                    k.mm(bg[:, :], [(Wg[i][:, c, fc * 128:(fc + 1) * 128], u2[:, c, gr * 512:(gr + 1) * 512]) for c in range(8)], bg, [Wg[i], u2])
                    bu = ngu()
                    k.mm(bu[:, :], [(Wu[i][:, c, fc * 128:(fc + 1) * 128], u2[:, c, gr * 512:(gr + 1) * 512]) for c in range(8)], bu, [Wu[i], u2])
                    sg_ = sgb[fc]
                    k.act(sg_[:, :], bg[:, :], AF.Silu, sg_, [bg])
                    k.tt("dve", H[:, fc, :], sg_[:, :], bu[:, :], ALU.mult, H, [sg_, bu])

            def down(e, gr, it):
                i = e % 2
                H = hT[it % 2]
                for t in range(4):
                    tile = gr * 4 + t
                    bd = ndn()
                    for hf in range(2):
                        k.mm(bd[:, hf * 512:(hf + 1) * 512], [(H[:, fc, t * 128:(t + 1) * 128], Wd[i][:, fc, hf * 512:(hf + 1) * 512]) for fc in range(2)],
                             bd, [H, Wd[i]], signal=(hf == 1))
                    k.op("dve", lambda v, tile=tile, bd=bd: v.scalar_tensor_tensor(out=acc[:, tile, :], in0=bd[:, :], scalar=cmb[:, tile, e:e + 1],
                                                                                  in1=acc[:, tile, :], op0=ALU.mult, op1=ALU.add),
                         reads=[bd, cmb, acc], writes=[acc])

            load_w(0)
            for it, (e, gr) in enumerate(seq):
                gate_up(e, gr, it)
                if pend is not None:
                    down(*pend)
                pend = (e, gr, it)
                if gr == 0 and e + 1 < NE:
                    load_w(e + 1)
            down(*pend)
            for t in range(16):
                for hf in range(2):
                    k.op("dve", lambda v, t=t, hf=hf: v.bn_stats(out=stt[:, hf * 6:(hf + 1) * 6], in_=acc[:, t, hf * 512:(hf + 1) * 512]),
                         reads=[acc], writes=[stt])
                k.op("dve", lambda v, t=t: v.bn_aggr(out=mv[:, t, :], in_=stt[:, :].rearrange("p (a b) -> p a b", b=6)),
                     reads=[stt], writes=[mv])
            for t in range(16):
                k.act(sd[:, t, 0:1], mv[:, t, 1:2], AF.Sqrt, sd, [mv], scale=1.0, bias=1e-5)
                k.op("dve", lambda v, t=t: v.reciprocal(sd[:, t, 1:2], sd[:, t, 0:1]), reads=[sd], writes=[sd])
                y_ = yo[t % 2]
                k.ts(y_[:, :], acc[:, t, :], mv[:, t, 0:1], sd[:, t, 1:2], ALU.subtract, ALU.mult, y_, [acc, mv, sd])
                k.tt("pool", y_[:, :], y_[:, :], l2g[:, :], ALU.mult, y_, [y_, l2g])
                k.tt("pool", y_[:, :], y_[:, :], l2b[:, :], ALU.add, y_, [y_, l2b])
                k.dma("sp", out_d[T0 + t * 128:T0 + (t + 1) * 128, :], y_[:, :], reads=[y_])
            k.barrier()
    return k, st


def _prep_inputs(inputs):
    f = lambda a: np.ascontiguousarray(np.asarray(a))
    x = f(inputs["x"])[:, ::-1, :]
    pos = f(inputs["positions"])[:, ::-1]
    inv = (1.0 / (10000.0 ** (np.arange(0, 32, 2, dtype=np.float32) / 32.0))).astype(np.float32)
    cst = np.zeros((128, 4), np.float32)
    cst[64:80, 0] = inv
    cst[80:96, 0] = inv
    cst[64:80, 1] = -1.0
    cst[80:96, 1] = 1.0
    shared = {"cst": cst}
    for name in ("w_ada", "b_ada", "w_in", "mla_q_norm_g", "w_q_up", "mla_kv_norm_g", "w_kv_up", "w_branch_sb",
                 "w_branch_mla", "w_out", "ln1_g", "ln1_b", "w_router_group", "b_router_group", "w_router_expert",
                 "b_router_expert", "w_exp_gate", "w_exp_up", "w_exp_down", "ln2_g", "ln2_b"):
        a = f(inputs[name])
        a = a.reshape(a.shape[1:]) if a.ndim >= 3 else a
        shared[name] = np.ascontiguousarray(a)
    maps = []
    for b in range(8):
        m = dict(shared)
        m["x"] = np.ascontiguousarray(x[b])
        m["c"] = np.ascontiguousarray(f(inputs["c"])[b:b + 1])
        m["positions"] = np.ascontiguousarray(pos[b:b + 1]).astype(np.int32)
        maps.append(m)
    return maps


def kernel(**inputs):
    maps = _prep_inputs(inputs)
    k, st = build()
    res = run_bass_kernel_spmd(k.nc, maps, core_ids=list(range(8)))
    out = np.stack([np.asarray(r["out"]) for r in res.results], axis=0)
    return np.ascontiguousarray(out[:, ::-1, :]).astype(np.float32)
```
